# Optimizing a Trainium2 kernel written in Bass

```python
import math
import jax, jax.numpy as jnp
from jax import lax
import numpy as np

D_MODEL = 1024
BATCH = 2
SEQ = 16384
DEPTH = 2

HG_HEADS = 4
HG_KEY_DIM = 128
HG_VAL_DIM = 64
HG_KEY_WIDTH = HG_HEADS * HG_KEY_DIM
HG_WIDTH = HG_HEADS * HG_VAL_DIM
HG_CHUNK = 64
MIN_FORGET = 1e-20

MLA_HEADS = 4
MLA_Q_RANK = 256
MLA_KV_RANK = 128
MLA_NOPE = 128
MLA_ROPE = 64
MLA_V = 128
MLA_WIDTH = MLA_HEADS * MLA_V
ROPE_THETA = 10000.0
ATTN_BLOCK = 128
MASK_VALUE = -1e30

POOL_GROUPS = 4
POOL_WINDOWS = (2, 4, 8, 16)
POOL_WIDTH = 256
POOL_GROUP_DIM = POOL_WIDTH // POOL_GROUPS

D_MIX = HG_WIDTH + MLA_WIDTH + POOL_WIDTH

IN_SIZES = (HG_KEY_WIDTH, HG_KEY_WIDTH, HG_WIDTH, HG_WIDTH,
            MLA_Q_RANK, MLA_KV_RANK, MLA_ROPE, POOL_WIDTH)
D_IN = sum(IN_SIZES)
IN_SPLITS = tuple(int(v) for v in np.cumsum(IN_SIZES)[:-1])

D_FF = 2560
N_EXPERTS = 8
TOP_K = 2
D_FF_EXPERT = 3584
N_DENSE = (DEPTH + 1) // 2
N_MOE = DEPTH // 2
EPS = 1e-6

kernel_name = "hybrid_hgrn2_mla_pool_moe"


def rms_norm(x, g):
    xf = x.astype(jnp.float32)
    y = xf * lax.rsqrt(jnp.mean(xf * xf, axis=-1, keepdims=True) + EPS)
    return (y * g.astype(jnp.float32)).astype(x.dtype)


def swiglu(x, wg, wu, wd):
    return (jax.nn.silu(x @ wg) * (x @ wu)) @ wd


def rope_tables(seq, dim):
    pos = jnp.arange(seq, dtype=jnp.float32)
    inv_freq = 1.0 / (ROPE_THETA ** (jnp.arange(0, dim, 2, dtype=jnp.float32) / dim))
    ang = pos[:, None] * inv_freq[None, :]
    return jnp.cos(ang), jnp.sin(ang)


def apply_rope(x, cos, sin):
    xf = x.astype(jnp.float32)
    x1, x2 = jnp.split(xf, 2, axis=-1)
    out = jnp.concatenate([x1 * cos - x2 * sin, x1 * sin + x2 * cos], axis=-1)
    return out.astype(x.dtype)


def hgrn2_mixer(q, f, i, g, lb, out_norm):
    B, S, _ = q.shape
    dt = q.dtype
    nc = S // HG_CHUNK
    lbf = lb.astype(jnp.float32)
    z = f.astype(jnp.float32)
    forget = lbf + (1.0 - lbf) * jax.nn.sigmoid(z)
    log_f = jnp.log(jnp.maximum(forget, MIN_FORGET))
    k = (1.0 - lbf) * jax.nn.sigmoid(-z)
    qf = jax.nn.silu(q.astype(jnp.float32))
    vf = i.astype(jnp.float32)

    def to_chunks(t, d):
        return t.reshape(B, nc, HG_CHUNK, HG_HEADS, d).transpose(1, 0, 3, 2, 4)

    qc = to_chunks(qf, HG_KEY_DIM)
    kc = to_chunks(k, HG_KEY_DIM)
    gc = to_chunks(log_f, HG_KEY_DIM)
    vc = to_chunks(vf, HG_VAL_DIM)
    tri = jnp.tril(jnp.ones((HG_CHUNK, HG_CHUNK), dtype=bool))

    def step(state, inp):
        qq, kk, vv, lg = inp
        b = jnp.cumsum(lg, axis=2)
        o_inter = jnp.einsum('bhtk,bhkv->bhtv', qq * jnp.exp(b), state)
        diff = b[:, :, :, None, :] - b[:, :, None, :, :]
        decay = jnp.exp(jnp.where(tri[:, :, None], diff, MASK_VALUE))
        attn = jnp.einsum('bhtk,bhsk,bhtsk->bhts', qq, kk, decay)
        o_intra = jnp.einsum('bhts,bhsv->bhtv', attn, vv)
        b_last = b[:, :, -1:, :]
        new_state = (jnp.exp(b_last[:, :, 0, :])[..., None] * state
                     + jnp.einsum('bhsk,bhsv->bhkv', kk * jnp.exp(b_last - b), vv))
        return new_state, o_inter + o_intra

    s0 = jnp.zeros((B, HG_HEADS, HG_KEY_DIM, HG_VAL_DIM), jnp.float32)
    _, o = lax.scan(step, s0, (qc, kc, vc, gc))
    o = o.transpose(1, 0, 3, 2, 4).reshape(B, S, HG_HEADS, HG_VAL_DIM)
    o = rms_norm(o, out_norm.reshape(HG_HEADS, HG_VAL_DIM))
    o = o.reshape(B, S, HG_WIDTH) * jax.nn.silu(g.astype(jnp.float32))
    return o.astype(dt)


def mla_mixer(c_q, c_kv, k_pe, q_norm, w_uq, kv_norm, w_ukv, cos, sin):
    B, S, _ = c_q.shape
    q = (rms_norm(c_q, q_norm) @ w_uq).reshape(B, S, MLA_HEADS, MLA_NOPE + MLA_ROPE)
    q_nope, q_pe = q[..., :MLA_NOPE], q[..., MLA_NOPE:]
    q_pe = apply_rope(q_pe, cos[:, None, :], sin[:, None, :])
    kv = (rms_norm(c_kv, kv_norm) @ w_ukv).reshape(B, S, MLA_HEADS, MLA_NOPE + MLA_V)
    k_nope, v = kv[..., :MLA_NOPE], kv[..., MLA_NOPE:]
    k_pe = apply_rope(k_pe, cos, sin)
    scale = (MLA_NOPE + MLA_ROPE) ** -0.5
    nb = S // ATTN_BLOCK
    qn_b = q_nope.reshape(B, nb, ATTN_BLOCK, MLA_HEADS, MLA_NOPE).transpose(1, 0, 2, 3, 4)
    qp_b = q_pe.reshape(B, nb, ATTN_BLOCK, MLA_HEADS, MLA_ROPE).transpose(1, 0, 2, 3, 4)
    kpos = jnp.arange(S)

    def attend(args):
        qn, qp, blk = args
        s = (jnp.einsum('bqhd,bkhd->bhqk', qn, k_nope)
             + jnp.einsum('bqhd,bkd->bhqk', qp, k_pe)).astype(jnp.float32) * scale
        qpos = blk * ATTN_BLOCK + jnp.arange(ATTN_BLOCK)
        s = jnp.where(qpos[:, None] >= kpos[None, :], s, MASK_VALUE)
        p = jax.nn.softmax(s, axis=-1).astype(v.dtype)
        return jnp.einsum('bhqk,bkhd->bqhd', p, v)

    out = lax.map(attend, (qn_b, qp_b, jnp.arange(nb)))
    return out.transpose(1, 0, 2, 3, 4).reshape(B, S, MLA_WIDTH)


def pool_mixer(xp, w_pool, scale):
    B, S, _ = xp.shape
    xf = xp.astype(jnp.float32)
    cs = jnp.cumsum(xf, axis=1)
    t = jnp.arange(S)
    outs = []
    for gi, w in enumerate(POOL_WINDOWS):
        c = cs[..., gi * POOL_GROUP_DIM:(gi + 1) * POOL_GROUP_DIM]
        lag = jnp.pad(c, ((0, 0), (w, 0), (0, 0)))[:, :S]
        cnt = jnp.minimum(t + 1, w).astype(jnp.float32)[None, :, None]
        outs.append((c - lag) / cnt)
    pooled = (jnp.concatenate(outs, axis=-1) - xf).astype(xp.dtype)
    y = jnp.einsum('bsgc,gcd->bsgd', pooled.reshape(B, S, POOL_GROUPS, POOL_GROUP_DIM), w_pool)
    return y.reshape(B, S, POOL_WIDTH) * scale


def moe_ffn(h, router, wg, wu, wd):
    B, S, D = h.shape
    tok = h.reshape(B * S, D)
    logits = (tok @ router).astype(jnp.float32)
    top_v, top_i = lax.top_k(logits, TOP_K)
    gates = jax.nn.softmax(top_v, axis=-1)
    combine = jnp.sum(jax.nn.one_hot(top_i, N_EXPERTS, dtype=jnp.float32) * gates[..., None], axis=1)
    out = jnp.zeros((B * S, D), jnp.float32)
    for e in range(N_EXPERTS):
        y = swiglu(tok, wg[e], wu[e], wd[e]).astype(jnp.float32)
        out = out + combine[:, e:e + 1] * y
    return out.reshape(B, S, D).astype(h.dtype)


def setup_inputs(seed: int = 0) -> dict:
    key = jax.random.key(seed)
    ks = jax.random.split(key, 24)
    f32 = jnp.float32

    def nrm(k, shape, fan_in):
        return jax.random.normal(k, shape, f32) * (fan_in ** -0.5)

    def gain(k, shape):
        return 1.0 + 0.1 * jax.random.normal(k, shape, f32)

    return {
        "x": jax.random.normal(ks[0], (BATCH, SEQ, D_MODEL), f32),
        "attn_norm": gain(ks[1], (DEPTH, D_MODEL)),
        "w_in": nrm(ks[2], (DEPTH, D_MODEL, D_IN), D_MODEL),
        "hgrn_lower_bounds": jax.random.normal(ks[3], (DEPTH, HG_KEY_WIDTH), f32),
        "hgrn_out_norm": gain(ks[4], (DEPTH, HG_WIDTH)),
        "mla_q_norm": gain(ks[5], (DEPTH, MLA_Q_RANK)),
        "mla_w_uq": nrm(ks[6], (DEPTH, MLA_Q_RANK, MLA_HEADS * (MLA_NOPE + MLA_ROPE)), MLA_Q_RANK),
        "mla_kv_norm": gain(ks[7], (DEPTH, MLA_KV_RANK)),
        "mla_w_ukv": nrm(ks[8], (DEPTH, MLA_KV_RANK, MLA_HEADS * (MLA_NOPE + MLA_V)), MLA_KV_RANK),
        "pool_w": nrm(ks[9], (DEPTH, POOL_GROUPS, POOL_GROUP_DIM, POOL_GROUP_DIM), POOL_GROUP_DIM),
        "pool_scale": gain(ks[10], (DEPTH, POOL_WIDTH)),
        "w_o": nrm(ks[11], (DEPTH, D_MIX, D_MODEL), D_MIX),
        "ffn_norm": gain(ks[12], (DEPTH, D_MODEL)),
        "dense_w_gate": nrm(ks[13], (N_DENSE, D_MODEL, D_FF), D_MODEL),
        "dense_w_up": nrm(ks[14], (N_DENSE, D_MODEL, D_FF), D_MODEL),
        "dense_w_down": nrm(ks[15], (N_DENSE, D_FF, D_MODEL), D_FF),
        "moe_router": nrm(ks[16], (N_MOE, D_MODEL, N_EXPERTS), D_MODEL),
        "moe_w_gate": nrm(ks[17], (N_MOE, N_EXPERTS, D_MODEL, D_FF_EXPERT), D_MODEL),
        "moe_w_up": nrm(ks[18], (N_MOE, N_EXPERTS, D_MODEL, D_FF_EXPERT), D_MODEL),
        "moe_w_down": nrm(ks[19], (N_MOE, N_EXPERTS, D_FF_EXPERT, D_MODEL), D_FF_EXPERT),
        "final_norm": gain(ks[20], (D_MODEL,)),
    }


def reference(x, attn_norm, w_in, hgrn_lower_bounds, hgrn_out_norm, mla_q_norm, mla_w_uq,
              mla_kv_norm, mla_w_ukv, pool_w, pool_scale, w_o, ffn_norm, dense_w_gate,
              dense_w_up, dense_w_down, moe_router, moe_w_gate, moe_w_up, moe_w_down,
              final_norm):
    S = x.shape[1]
    cos, sin = rope_tables(S, MLA_ROPE)
    p_lb = jax.nn.softmax(hgrn_lower_bounds.astype(jnp.float32), axis=0)
    lbs = jnp.cumsum(p_lb, axis=0) - p_lb[0:1]

    for l in range(DEPTH):
        h = rms_norm(x, attn_norm[l])
        proj = h @ w_in[l]
        hq, hf, hi, hg, c_q, c_kv, k_pe, xp = jnp.split(proj, IN_SPLITS, axis=-1)
        o_a = hgrn2_mixer(hq, hf, hi, hg, lbs[l], hgrn_out_norm[l])
        o_b = mla_mixer(c_q, c_kv, k_pe, mla_q_norm[l], mla_w_uq[l], mla_kv_norm[l],
                        mla_w_ukv[l], cos, sin)
        o_c = pool_mixer(xp, pool_w[l], pool_scale[l])
        x = x + jnp.concatenate([o_a, o_b, o_c], axis=-1) @ w_o[l]
        h = rms_norm(x, ffn_norm[l])
        if l % 2 == 0:
            j = l // 2
            x = x + swiglu(h, dense_w_gate[j], dense_w_up[j], dense_w_down[j])
        else:
            j = l // 2
            x = x + moe_ffn(h, moe_router[j], moe_w_gate[j], moe_w_up[j], moe_w_down[j])
    return rms_norm(x, final_norm)
```

```python
import numpy as np
from contextlib import ExitStack
import ml_dtypes
import concourse.bass as bass
import concourse.mybir as mybir
from concourse.bass_utils import run_bass_kernel_spmd

F32 = mybir.dt.float32
BF16 = mybir.dt.bfloat16
AF = mybir.ActivationFunctionType
ALU = mybir.AluOpType
AX = mybir.AxisListType

NCORES = 8
D = 1024
SEQ = 16384
BATCH = 2
TOK = 4096
DFF = 2560
DFFE = 3584
NEXP = 8
EPS = 1e-6
D_IN = 2240


class Buf:
    __slots__ = ("name", "w", "r")

    def __init__(self, name=""):
        self.name = name
        self.w = None
        self.r = []


class Chan:
    __slots__ = ("sem", "total", "id")


class Sync:
    def __init__(self, nc, stack):
        self.nc = nc
        self.stack = stack
        self.eng = {"pe": nc.tensor, "act": nc.scalar, "dve": nc.vector,
                    "pool": nc.gpsimd, "sp": nc.sync}
        self.sem = {}
        self.cnt = {}
        self.seen = {}
        for k in self.eng:
            self.sem[k] = stack.enter_context(nc.semaphore("s_" + k))
            self.cnt[k] = 0
            self.seen[k] = {}
        self.chans = []

    def chan(self, name=""):
        c = Chan()
        c.sem = self.stack.enter_context(self.nc.semaphore("d%d_%s" % (len(self.chans), name)))
        c.total = 0
        c.id = len(self.chans)
        self.chans.append(c)
        return c

    def _wait(self, e, ev):
        kind, key, val = ev
        if kind == "e" and key == e and e == "pe":
            return
        k = (kind, key if kind == "e" else key.id)
        if self.seen[e].get(k, 0) >= val:
            return
        self.seen[e][k] = val
        sem = self.sem[key] if kind == "e" else key.sem
        self.eng[e].wait_ge(sem, val)

    def _deps(self, e, reads, writes):
        for b in reads:
            if b.w is not None:
                self._wait(e, b.w)
        for b in writes:
            if b.w is not None:
                self._wait(e, b.w)
            for ev in b.r:
                if ev[0] == "e" and ev[1] == e:
                    continue
                self._wait(e, ev)

    def _mark(self, ev, reads, writes):
        for b in writes:
            b.w = ev
            b.r = []
        for b in reads:
            if b in writes:
                continue
            b.r = [x for x in b.r if not (x[0] == ev[0] and x[1] is ev[1])]
            b.r.append(ev)

    def op(self, e, reads, writes, fn, inc=True):
        self._deps(e, reads, writes)
        ins = fn(self.eng[e])
        if inc:
            ins.then_inc(self.sem[e], 1)
            self.cnt[e] += 1
            ev = ("e", e, self.cnt[e])
        else:
            ev = ("e", e, self.cnt[e] + 1)
        self._mark(ev, reads, writes)
        return ins

    def dma(self, q, ch, pairs, reads, writes, **kw):
        self._deps(q, reads, writes)
        for (o, i) in pairs:
            ins = self.eng[q].dma_start(out=o, in_=i, **kw)
            ins.then_inc(ch.sem, 16)
            ch.total += 16
        ev = ("d", ch, ch.total)
        self._mark(ev, reads, writes)

    def barrier(self):
        for e in self.eng:
            for f in self.eng:
                if f != e and self.cnt[f] > 0:
                    self._wait(e, ("e", f, self.cnt[f]))
            for c in self.chans:
                if c.total > 0:
                    self._wait(e, ("d", c, c.total))

    def finish(self, e="sp"):
        for c in self.chans:
            if c.total > 0:
                self._wait(e, ("d", c, c.total))
        for f in self.eng:
            if f != e and self.cnt[f] > 0:
                self._wait(e, ("e", f, self.cnt[f]))


class Ring:
    def __init__(self, items):
        self.items = items
        self.i = 0

    def next(self):
        it = self.items[self.i % len(self.items)]
        self.i += 1
        return it


class Ctx:
    def __init__(self, nc, stack):
        self.nc = nc
        self.st = stack
        self.S = Sync(nc, stack)
        self.n = 0

    def sb(self, shape, dt, name=None):
        self.n += 1
        t = self.st.enter_context(self.nc.sbuf_tensor("S_" + (name or ("sb%d" % self.n)), list(shape), dt))
        return t

    def ps(self, shape, dt=F32, name=None):
        self.n += 1
        t = self.st.enter_context(self.nc.psum_tensor("P_" + (name or ("ps%d" % self.n)), list(shape), dt))
        return t

    def sb_ring(self, n, shape, dt, name):
        return Ring([(self.sb(shape, dt, "%s%d" % (name, i)), Buf("%s%d" % (name, i))) for i in range(n)])

    def ps_ring(self, n, shape, dt, name):
        return Ring([(self.ps(shape, dt, "%s%d" % (name, i)), Buf("%s%d" % (name, i))) for i in range(n)])


def emit_rmsnorm(C, K, xs, B_xs, g_sb, out_fn, NT):
    S = C.S
    for tt in range(NT // 512):
        sl = slice(tt * 512, (tt + 1) * 512)
        sq, B_sq = K["sq"].next()
        for oc in range(8):
            S.op("act", [B_xs[oc][tt]], [B_sq],
                 lambda e: e.activation(out=sq[:, oc, :], in_=xs[:, oc, sl], func=AF.Square))
        pss, B_pss = K["pmisc"].next()
        for oc in range(8):
            S.op("pe", [B_sq, K["B_const"]], [B_pss],
                 lambda e: e.matmul(pss[:], lhsT=K["ones_bf"][:], rhs=sq[:, oc, :],
                                    start=(oc == 0), stop=(oc == 7)), inc=(oc == 7))
        rstd, B_rstd = K["rstd"].next()
        S.op("act", [B_pss, K["B_const"]], [B_rstd],
             lambda e: e.activation(out=rstd[:], in_=pss[:], func=AF.Ln, scale=1.0 / D,
                                    bias=K["eps"][:, 0:1]))
        S.op("act", [B_rstd], [B_rstd],
             lambda e: e.activation(out=rstd[:], in_=rstd[:], func=AF.Exp, scale=-0.5))
        for oc in range(8):
            o_ap, wb = out_fn(oc, tt)
            S.op("dve", [B_xs[oc][tt], B_rstd, K["B_const"]], wb,
                 lambda e: e.scalar_tensor_tensor(out=o_ap, in0=xs[:, oc, sl], scalar=g_sb[:, oc:oc + 1],
                                                  in1=rstd[:], op0=ALU.mult, op1=ALU.mult))


def emit_down(C, K, src, B_src, nf, wd, xs, B_xs, NT, bc=None, B_bc=None):
    S = C.S
    for oc in range(8):
        dsl, B_d = K["dslot"].next()
        S.dma("pool", K["ch_d"][(K["dslot"].i - 1) % len(K["ch_d"])],
              [(dsl[:, 0:nf, :], wd[:, oc * 128:(oc + 1) * 128].rearrange("(f p) n -> p f n", p=128))],
              [], [B_d])
        for tt in range(NT // 512):
            sl = slice(tt * 512, (tt + 1) * 512)
            py, B_py = K["py"].next()
            for f in range(nf):
                S.op("pe", [B_d, B_src[tt]], [B_py],
                     lambda e: e.matmul(py[:], lhsT=dsl[:, f, :], rhs=src[:, f, sl],
                                        start=(f == 0), stop=(f == nf - 1)), inc=(f == nf - 1))
            if bc is None:
                S.op("dve", [B_py, B_xs[oc][tt]], [B_xs[oc][tt]],
                     lambda e: e.tensor_tensor(out=xs[:, oc, sl], in0=py[:], in1=xs[:, oc, sl], op=ALU.add))
            else:
                tmp, B_tmp = K["sil"].next()
                S.op("dve", [B_py, B_bc], [B_tmp],
                     lambda e: e.tensor_tensor(out=tmp[:], in0=py[:], in1=bc[:, sl], op=ALU.mult))
                S.op("dve", [B_tmp, B_xs[oc][tt]], [B_xs[oc][tt]],
                     lambda e: e.tensor_tensor(out=xs[:, oc, sl], in0=tmp[:], in1=xs[:, oc, sl], op=ALU.add))


def emit_gate_up(C, K, h2, B_h2, wg, wu, nf, a, B_a, NT):
    S = C.S
    for f in range(nf):
        gu, B_gu = K["guslot"].next()
        S.dma("pool", K["ch_gu"][(K["guslot"].i - 1) % len(K["ch_gu"])],
              [(gu[:, :, 0:128], wg[:, f * 128:(f + 1) * 128].rearrange("(k p) n -> p k n", p=128)),
               (gu[:, :, 128:256], wu[:, f * 128:(f + 1) * 128].rearrange("(k p) n -> p k n", p=128))],
              [], [B_gu])
        for tt in range(NT // 512):
            sl = slice(tt * 512, (tt + 1) * 512)
            pg, B_pg = K["pg"].next()
            pu, B_pu = K["pu"].next()
            for k in range(8):
                S.op("pe", [B_gu, B_h2[tt]], [B_pg],
                     lambda e: e.matmul(pg[:], lhsT=gu[:, k, 0:128], rhs=h2[:, k, sl],
                                        start=(k == 0), stop=(k == 7)), inc=(k == 7))
            for k in range(8):
                S.op("pe", [B_gu, B_h2[tt]], [B_pu],
                     lambda e: e.matmul(pu[:], lhsT=gu[:, k, 128:256], rhs=h2[:, k, sl],
                                        start=(k == 0), stop=(k == 7)), inc=(k == 7))
            sil, B_sil = K["sil"].next()
            S.op("act", [B_pg], [B_sil],
                 lambda e: e.activation(out=sil[:], in_=pg[:], func=AF.Silu))
            S.op("dve", [B_sil, B_pu], [B_a[tt]],
                 lambda e: e.tensor_tensor(out=a[:, f, sl], in0=pu[:], in1=sil[:], op=ALU.mult))


def emit_router(C, K, h2, B_h2, rt_sb, combT, B_combT, NT):
    S = C.S
    nc = C.nc
    for s in range(NT // 128):
        tt = s // 4
        tsl = slice(s * 128, (s + 1) * 128)
        pl, B_pl = K["pmisc"].next()
        for k in range(8):
            S.op("pe", [B_h2[tt], K["B_const"]], [B_pl],
                 lambda e: e.matmul(pl[:, 0:8], lhsT=h2[:, k, tsl], rhs=rt_sb[:, k, :],
                                    start=(k == 0), stop=(k == 7)), inc=(k == 7))
        r, B_r = K["rt"].next()
        S.op("act", [B_pl], [B_r], lambda e: e.activation(out=r[:, 0:8], in_=pl[:, 0:8], func=AF.Copy))
        S.op("dve", [B_r], [B_r], lambda e: e.max(out=r[:, 8:16], in_=r[:, 0:8]))
        S.op("dve", [B_r], [B_r], lambda e: e.tensor_scalar(out=r[:, 16:24], in0=r[:, 0:8], scalar1=r[:, 9:10],
                                                            scalar2=None, op0=ALU.is_ge))
        S.op("dve", [B_r], [B_r], lambda e: e.tensor_scalar(out=r[:, 32:33], in0=r[:, 8:9], scalar1=-1.0,
                                                            scalar2=None, op0=ALU.mult))
        S.op("act", [B_r], [B_r], lambda e: e.activation(out=r[:, 24:32], in_=r[:, 0:8], func=AF.Exp,
                                                         bias=r[:, 32:33], scale=1.0))
        S.op("dve", [B_r], [B_r], lambda e: e.tensor_tensor(out=r[:, 24:32], in0=r[:, 24:32], in1=r[:, 16:24],
                                                            op=ALU.mult))
        S.op("dve", [B_r], [B_r], lambda e: e.reduce_sum(out=r[:, 33:34], in_=r[:, 24:32], axis=AX.X))
        S.op("dve", [B_r], [B_r], lambda e: e.reciprocal(out=r[:, 34:35], in_=r[:, 33:34]))
        S.op("dve", [B_r], [B_r], lambda e: e.tensor_scalar(out=r[:, 40:48], in0=r[:, 24:32], scalar1=r[:, 34:35],
                                                            scalar2=None, op0=ALU.mult))
        pt, B_pt = K["pmisc"].next()
        S.op("pe", [B_r, K["B_const"]], [B_pt],
             lambda e: e.transpose(out=pt[0:8, 0:128], in_=r[:, 40:48], identity=K["ident"][:]))
        S.op("act", [B_pt], [B_combT],
             lambda e: e.activation(out=combT[0:8, tsl], in_=pt[0:8, 0:128], func=AF.Copy))


def build_C(kind, final, NT=1024):
    nc = bass.Bass("TRN2", target_bir_lowering=False)
    dt = nc.dram_tensor
    xT = dt("xT", [D, TOK], F32, kind="ExternalInput").ap()
    oT = dt("oT", [D, TOK], BF16, kind="ExternalInput").ap()
    wo = dt("wo", [D, D], F32, kind="ExternalInput").ap()
    gvec = dt("gvec", [128, 16], F32, kind="ExternalInput").ap()
    ident_d = dt("ident", [128, 128], F32, kind="ExternalInput").ap()
    if kind == "dense":
        wg = dt("wg", [D, DFF], F32, kind="ExternalInput").ap()
        wu = dt("wu", [D, DFF], F32, kind="ExternalInput").ap()
        wd = dt("wd", [DFF, D], F32, kind="ExternalInput").ap()
        nfmax = DFF // 128
    else:
        wg = dt("wg", [NEXP, D, DFFE], F32, kind="ExternalInput").ap()
        wu = dt("wu", [NEXP, D, DFFE], F32, kind="ExternalInput").ap()
        wd = dt("wd", [NEXP, DFFE, D], F32, kind="ExternalInput").ap()
        rt = dt("router", [D, NEXP], F32, kind="ExternalInput").ap()
        nfmax = DFFE // 128
    if final:
        outT = dt("outT", [D, TOK], F32, kind="ExternalOutput").ap()
    else:
        x2T = dt("x2T", [D, TOK], F32, kind="ExternalOutput").ap()
        hnT = dt("hnT", [D, TOK], BF16, kind="ExternalOutput").ap()

    with ExitStack() as st:
        C = Ctx(nc, st)
        S = C.S
        TT = NT // 512
        K = {}
        K["B_const"] = Buf("const")
        K["ones_bf"] = C.sb([128, 128], BF16, "ones_bf")
        K["eps"] = C.sb([128, 1], F32, "eps")
        K["ident"] = C.sb([128, 128], F32, "ident")
        g_sb = C.sb([128, 16], F32, "g_sb")
        ch_c = S.chan("const")
        S.op("dve", [], [K["B_const"]], lambda e: e.memset(K["ones_bf"][:], 1.0))
        S.op("dve", [], [K["B_const"]], lambda e: e.memset(K["eps"][:], EPS))
        S.dma("sp", ch_c, [(g_sb[:], gvec), (K["ident"][:], ident_d)], [], [K["B_const"]])
        if kind == "moe":
            rt_sb = C.sb([128, 8, 8], BF16, "rt_sb")
            S.dma("pool", ch_c, [(rt_sb[:], rt.rearrange("(k p) e -> p k e", p=128))], [], [K["B_const"]])
            esel = C.sb([8, 8, 128], F32, "esel")
            for e_ in range(8):
                S.op("dve", [K["B_const"]], [K["B_const"]],
                     lambda e: e.tensor_copy(out=esel[0:8, e_, :],
                                             in_=K["ident"][0:8, e_:e_ + 1].to_broadcast([8, 128])))
            combT = C.sb([8, NT], F32, "combT")
            B_combT = Buf("combT")
            bc = C.sb([128, NT], F32, "bc")
            B_bc = Buf("bc")
            K["rt"] = C.sb_ring(2, [128, 48], F32, "rts")
        xs = C.sb([128, 8, NT], F32, "xs")
        B_xs = [[Buf("xs%d_%d" % (oc, tt)) for tt in range(TT)] for oc in range(8)]
        osb = C.sb([128, 8, NT], BF16, "osb")
        B_os = [Buf("os%d" % tt) for tt in range(TT)]
        h2 = C.sb([128, 8, NT], BF16, "h2")
        B_h2 = [Buf("h2_%d" % tt) for tt in range(TT)]
        a = C.sb([128, nfmax, NT], BF16, "a")
        B_a = [Buf("a%d" % tt) for tt in range(TT)]
        K["sq"] = C.sb_ring(2, [128, 8, 512], BF16, "sq")
        K["rstd"] = C.sb_ring(2, [128, 512], F32, "rstd")
        K["sil"] = C.sb_ring(3, [128, 512], F32, "sil")
        K["guslot"] = C.sb_ring(5, [128, 8, 256], BF16, "gu")
        K["ch_gu"] = [S.chan("gu%d" % i) for i in range(5)]
        K["dslot"] = C.sb_ring(3, [128, nfmax, 128], BF16, "dsl")
        K["ch_d"] = [S.chan("d%d" % i) for i in range(3)]
        K["pg"] = C.ps_ring(2, [128, 512], F32, "pg")
        K["pu"] = C.ps_ring(2, [128, 512], F32, "pu")
        K["py"] = C.ps_ring(2, [128, 512], F32, "py")
        K["pmisc"] = C.ps_ring(2, [128, 512], F32, "pm")
        ch_x = S.chan("x")
        ch_o = S.chan("o")
        ch_out = S.chan("out")
        if final:
            ostage = C.sb_ring(2, [128, 512], F32, "ost")
        else:
            hn = C.sb([128, 8, NT], BF16, "hn")
            B_hn = [Buf("hn%d" % tt) for tt in range(TT)]

        for sti in range(TOK // NT):
            t0 = sti * NT
            S.dma("sp", ch_x, [(xs[:, :, tt * 512:(tt + 1) * 512],
                                xT[:, t0 + tt * 512:t0 + (tt + 1) * 512].rearrange("(k p) t -> p k t", p=128))
                               for tt in range(TT)],
                  [], [B_xs[oc][tt] for oc in range(8) for tt in range(TT)])
            S.dma("sp", ch_o, [(osb[:, :, tt * 512:(tt + 1) * 512],
                                oT[:, t0 + tt * 512:t0 + (tt + 1) * 512].rearrange("(k p) t -> p k t", p=128))
                               for tt in range(TT)],
                  [], B_os)
            emit_down(C, K, osb, B_os, 8, wo, xs, B_xs, NT)
            emit_rmsnorm(C, K, xs, B_xs, g_sb, lambda oc, tt: (h2[:, oc, tt * 512:(tt + 1) * 512], [B_h2[tt]]), NT)
            if kind == "dense":
                emit_gate_up(C, K, h2, B_h2, wg, wu, DFF // 128, a, B_a, NT)
                emit_down(C, K, a, B_a, DFF // 128, wd, xs, B_xs, NT)
            else:
                emit_router(C, K, h2, B_h2, rt_sb, combT, B_combT, NT)
                for ex in range(NEXP):
                    for tt in range(TT):
                        sl = slice(tt * 512, (tt + 1) * 512)
                        pb, B_pb = K["pmisc"].next()
                        S.op("pe", [B_combT, K["B_const"]], [B_pb],
                             lambda e: e.matmul(pb[:], lhsT=esel[0:8, ex, :], rhs=combT[0:8, sl],
                                                start=True, stop=True))
                        S.op("act", [B_pb], [B_bc],
                             lambda e: e.activation(out=bc[:, sl], in_=pb[:], func=AF.Copy))
                    emit_gate_up(C, K, h2, B_h2, wg[ex], wu[ex], DFFE // 128, a, B_a, NT)
                    emit_down(C, K, a, B_a, DFFE // 128, wd[ex], xs, B_xs, NT, bc=bc, B_bc=B_bc)
            if final:
                def out_fn(oc, tt):
                    return None
                stage_list = {}

                def fin_out(oc, tt):
                    t_, b_ = ostage.next()
                    stage_list[(oc, tt)] = (t_, b_)
                    return (t_[:], [b_])
                _emit_final(C, K, xs, B_xs, g_sb, ostage, outT, t0, ch_out, NT)
            else:
                S.dma("sp", ch_out, [(x2T[:, t0 + tt * 512:t0 + (tt + 1) * 512].rearrange("(k p) t -> p k t", p=128),
                                      xs[:, :, tt * 512:(tt + 1) * 512]) for tt in range(TT)],
                      [B_xs[oc][tt] for oc in range(8) for tt in range(TT)], [])
                emit_rmsnorm(C, K, xs, B_xs, g_sb[:, 8:16],
                             lambda oc, tt: (hn[:, oc, tt * 512:(tt + 1) * 512], [B_hn[tt]]), NT)
                S.dma("sp", ch_out, [(hnT[:, t0 + tt * 512:t0 + (tt + 1) * 512].rearrange("(k p) t -> p k t", p=128),
                                      hn[:, :, tt * 512:(tt + 1) * 512]) for tt in range(TT)],
                      B_hn, [])
        S.finish("sp")
    return nc


def _emit_final(C, K, xs, B_xs, g_sb, ostage, outT, t0, ch_out, NT):
    S = C.S
    for tt in range(NT // 512):
        sl = slice(tt * 512, (tt + 1) * 512)
        sq, B_sq = K["sq"].next()
        for oc in range(8):
            S.op("act", [B_xs[oc][tt]], [B_sq],
                 lambda e: e.activation(out=sq[:, oc, :], in_=xs[:, oc, sl], func=AF.Square))
        pss, B_pss = K["pmisc"].next()
        for oc in range(8):
            S.op("pe", [B_sq, K["B_const"]], [B_pss],
                 lambda e: e.matmul(pss[:], lhsT=K["ones_bf"][:], rhs=sq[:, oc, :],
                                    start=(oc == 0), stop=(oc == 7)), inc=(oc == 7))
        rstd, B_rstd = K["rstd"].next()
        S.op("act", [B_pss, K["B_const"]], [B_rstd],
             lambda e: e.activation(out=rstd[:], in_=pss[:], func=AF.Ln, scale=1.0 / D, bias=K["eps"][:, 0:1]))
        S.op("act", [B_rstd], [B_rstd],
             lambda e: e.activation(out=rstd[:], in_=rstd[:], func=AF.Exp, scale=-0.5))
        for oc in range(8):
            ot, B_ot = ostage.next()
            S.op("dve", [B_xs[oc][tt], B_rstd, K["B_const"]], [B_ot],
                 lambda e: e.scalar_tensor_tensor(out=ot[:], in0=xs[:, oc, sl], scalar=g_sb[:, 8 + oc:9 + oc],
                                                  in1=rstd[:], op0=ALU.mult, op1=ALU.mult))
            S.dma("sp", ch_out, [(outT[oc * 128:(oc + 1) * 128, t0 + tt * 512:t0 + (tt + 1) * 512], ot[:])],
                  [B_ot], [])


def build_A():
    nc = bass.Bass("TRN2", target_bir_lowering=False)
    xT = nc.dram_tensor("xT", [D, TOK], F32, kind="ExternalInput").ap()
    gvec = nc.dram_tensor("gvec", [128, 16], F32, kind="ExternalInput").ap()
    hT = nc.dram_tensor("hT", [D, TOK], BF16, kind="ExternalOutput").ap()
    with ExitStack() as st:
        C = Ctx(nc, st)
        S = C.S
        K = {}
        K["B_const"] = Buf("const")
        K["ones_bf"] = C.sb([128, 128], BF16, "ones_bf")
        K["eps"] = C.sb([128, 1], F32, "eps")
        g_sb = C.sb([128, 16], F32, "g_sb")
        ch_c = S.chan("const")
        S.op("dve", [], [K["B_const"]], lambda e: e.memset(K["ones_bf"][:], 1.0))
        S.op("dve", [], [K["B_const"]], lambda e: e.memset(K["eps"][:], EPS))
        S.dma("sp", ch_c, [(g_sb[:], gvec)], [], [K["B_const"]])
        K["sq"] = C.sb_ring(2, [128, 8, 512], BF16, "sq")
        K["rstd"] = C.sb_ring(2, [128, 512], F32, "rstd")
        K["pmisc"] = C.ps_ring(2, [128, 512], F32, "pm")
        xr = C.sb_ring(2, [128, 8, 512], F32, "xs")
        hr = C.sb_ring(2, [128, 8, 512], BF16, "hs")
        chx = [S.chan("x0"), S.chan("x1")]
        cho = [S.chan("o0"), S.chan("o1")]
        for j in range(TOK // 512):
            xs, B = xr.next()
            hs, Bh = hr.next()
            S.dma("sp", chx[j % 2], [(xs[:], xT[:, j * 512:(j + 1) * 512].rearrange("(k p) t -> p k t", p=128))],
                  [], [B])
            B_xs = [[B] for _ in range(8)]
            emit_rmsnorm(C, K, xs, B_xs, g_sb, lambda oc, tt: (hs[:, oc, :], [Bh]), 512)
            S.dma("sp", cho[j % 2], [(hT[:, j * 512:(j + 1) * 512].rearrange("(k p) t -> p k t", p=128), hs[:])],
                  [Bh], [])
        S.finish("sp")
    return nc


SCALE = (128 + 64) ** -0.5


def build_B(NBLK=32):
    NS = NBLK * 512
    nc = bass.Bass("TRN2", target_bir_lowering=False)
    dt = nc.dram_tensor
    hT = dt("hT", [D, NS], BF16, kind="ExternalInput").ap()
    winh = dt("winh", [D, 1024], F32, kind="ExternalInput").ap()
    wuq = dt("wuq", [256, 256], F32, kind="ExternalInput").ap()
    wukv = dt("wukv", [128, 256], F32, kind="ExternalInput").ap()
    poolw = dt("poolw", [64, 64], F32, kind="ExternalInput").ap()
    vecs = dt("vecs", [128, 16], F32, kind="ExternalInput").ap()
    cs2 = dt("cs2", [64, 2, NS], F32, kind="ExternalInput").ap()
    band = dt("band", [128, 3, 128], F32, kind="ExternalInput").ap()
    consts = dt("consts", [128, 3, 512], F32, kind="ExternalInput").ap()
    oT = dt("oTo", [256, NS], BF16, kind="ExternalOutput").ap()

    with ExitStack() as st:
        C = Ctx(nc, st)
        S = C.S
        Bc = Buf("const")
        ch_c = S.chan("const")
        win = C.sb([128, 8, 1024], BF16, "win")
        S.dma("pool", ch_c, [(win[:, k, :], winh[k * 128:(k + 1) * 128, :]) for k in range(8)], [], [Bc])
        wuq_sb = C.sb([128, 2, 256], BF16, "wuq")
        S.dma("pool", ch_c, [(wuq_sb[:], wuq.rearrange("(k p) n -> p k n", p=128))], [], [Bc])
        wukv_sb = C.sb([128, 256], BF16, "wukv")
        poolw_sb = C.sb([64, 64], BF16, "poolw")
        band_sb = C.sb([128, 3, 128], BF16, "band")
        S.dma("pool", ch_c, [(wukv_sb[:], wukv), (poolw_sb[:], poolw), (band_sb[:], band)], [], [Bc])
        vec = C.sb([128, 16], F32, "vec")
        cst = C.sb([128, 3, 512], F32, "cst")
        S.dma("sp", ch_c, [(vec[:], vecs), (cst[:], consts)], [], [Bc])
        ident_bf = C.sb([128, 128], BF16, "identbf")
        tri_bf = C.sb([128, 128], BF16, "tribf")
        cmask = C.sb([128, 512], F32, "cmask")
        ones_bf = C.sb([128, 128], BF16, "onesbf")
        eps = C.sb([128, 1], F32, "eps")
        lbv = C.sb([128, 4], F32, "lbv")
        S.op("dve", [Bc], [Bc], lambda e: e.tensor_copy(out=ident_bf[:], in_=cst[:, 0, 0:128]))
        S.op("dve", [Bc], [Bc], lambda e: e.tensor_copy(out=tri_bf[:], in_=cst[:, 1, 0:128]))
        S.op("dve", [Bc], [Bc], lambda e: e.tensor_copy(out=cmask[:], in_=cst[:, 2, :]))
        S.op("dve", [], [Bc], lambda e: e.memset(ones_bf[:], 1.0))
        S.op("dve", [], [Bc], lambda e: e.memset(eps[:], EPS))
        S.op("dve", [Bc], [Bc], lambda e: e.tensor_tensor(out=lbv[:, 3:4], in0=vec[:, 2:3], in1=vec[:, 1:2],
                                                         op=ALU.subtract))
        S.op("act", [Bc], [Bc], lambda e: e.activation(out=lbv[:, 3:4], in_=lbv[:, 3:4], func=AF.Sigmoid))
        S.op("dve", [Bc], [Bc], lambda e: e.tensor_tensor(out=lbv[:, 0:1], in0=lbv[:, 3:4], in1=vec[:, 3:4],
                                                         op=ALU.mult))
        S.op("dve", [Bc], [Bc], lambda e: e.tensor_scalar(out=lbv[:, 1:2], in0=lbv[:, 0:1], scalar1=-1.0, scalar2=1.0,
                                                         op0=ALU.mult, op1=ALU.add))
        S.op("dve", [Bc], [Bc], lambda e: e.tensor_scalar(out=lbv[:, 2:3], in0=lbv[:, 1:2], scalar1=-1.0, scalar2=None,
                                                         op0=ALU.mult))
        KnT = C.sb([128, NS], BF16, "KnT")
        KpT = C.sb([64, NS], BF16, "KpT")
        Vt = C.sb([128, NS // 128, 128], BF16, "Vt")
        B_K = [Buf("K%d" % t) for t in range(NBLK)]
        Sst = C.sb([128, 64], F32, "Sst")
        Sbf = C.sb([128, 2, 64], BF16, "Sbf")
        B_S = Buf("state")
        B_Sb = [Buf("sb0"), Buf("sb1")]
        S.op("dve", [], [B_S], lambda e: e.memset(Sst[:], 0.0))
        S.op("dve", [], B_Sb, lambda e: e.memset(Sbf[:], 0.0))
        tri8 = C.sb([64, 512], BF16, "tri8")
        for c_ in range(8):
            S.op("dve", [Bc], [Bc], lambda e: e.tensor_copy(out=tri8[:, c_ * 64:(c_ + 1) * 64], in_=cst[0:64, 1, 0:64]))
        khtr = C.sb_ring(2, [64, 1024], BF16, "kht")
        hring = C.sb_ring(2, [128, 8, 512], BF16, "hblk")
        ch_h = [S.chan("h0"), S.chan("h1")]
        csring = C.sb_ring(2, [64, 2, 512], F32, "cs")
        ch_cs = [S.chan("cs0"), S.chan("cs1")]
        pA = C.ps_ring(3, [128, 512], F32, "pA")
        pS = C.ps_ring(2, [128, 512], F32, "pS")
        pO = C.ps([128, 512], F32, "pO")
        pL = C.ps([128, 512], F32, "pL")
        B_pO, B_pL = Buf("pO"), Buf("pL")
        pH = C.ps([128, 512], F32, "pH")
        B_pH = Buf("pH")
        pX = C.ps([128, 512], F32, "pX") if False else None
        f32r = C.sb_ring(12, [128, 512], F32, "f")
        bfr = C.sb_ring(8, [128, 512], BF16, "b")
        ptr = C.sb_ring(3, [128, 512], BF16, "pt")
        qr = C.sb_ring(2, [128, 2, 512], BF16, "q")
        xpr = C.sb_ring(3, [128, 64], BF16, "xp")
        outr = C.sb_ring(2, [128, 3, 512], BF16, "out")
        ch_out = [S.chan("out0"), S.chan("out1")]
        prev_xp = None

        def rms_rstd(src_list, npart, nfeat):
            sq, B_sq = bfr.next()
            pss, B_pss = pA.next()
            for i, (ap, B) in enumerate(src_list):
                S.op("act", [B], [B_sq], lambda e: e.activation(out=sq[0:npart, :], in_=ap, func=AF.Square))
                S.op("pe", [B_sq, Bc], [B_pss],
                     lambda e: e.matmul(pss[0:npart, :], lhsT=ones_bf[0:npart, 0:npart], rhs=sq[0:npart, :],
                                        start=(i == 0), stop=(i == len(src_list) - 1)))
            rstd, B_r = f32r.next()
            S.op("act", [B_pss, Bc], [B_r],
                 lambda e: e.activation(out=rstd[0:npart, :], in_=pss[0:npart, :], func=AF.Ln, scale=1.0 / nfeat,
                                        bias=eps[0:npart, 0:1]))
            S.op("act", [B_r], [B_r],
                 lambda e: e.activation(out=rstd[0:npart, :], in_=rstd[0:npart, :], func=AF.Exp, scale=-0.5))
            return rstd, B_r

        def proj(hb, B_hb, c0, m):
            p, B_p = pA.next()
            for k in range(8):
                S.op("pe", [B_hb, Bc], [B_p],
                     lambda e: e.matmul(p[0:m, :], lhsT=win[:, k, c0:c0 + m], rhs=hb[:, k, :],
                                        start=(k == 0), stop=(k == 7)), inc=(k == 7))
            return p, B_p

        for t in range(NBLK):
            bsl = slice(t * 512, (t + 1) * 512)
            hb, B_hb = hring.next()
            S.dma("sp", ch_h[t % 2], [(hb[:], hT[:, bsl].rearrange("(k p) t -> p k t", p=128))], [], [B_hb])
            cs, B_cs = csring.next()
            S.dma("sp", ch_cs[t % 2], [(cs[:], cs2[:, :, bsl])], [], [B_cs])
            ot, B_ot = outr.next()
            q, B_q = qr.next()

            pq0, B_pq0 = proj(hb, B_hb, 512, 128)
            pq1, B_pq1 = proj(hb, B_hb, 640, 128)
            rstd, B_r = rms_rstd([(pq0[:], B_pq0), (pq1[:], B_pq1)], 128, 256)
            cqn, B_cqn = bfr.next()
            cqn2, B_cqn2 = bfr.next()
            S.op("dve", [B_pq0, B_r, Bc], [B_cqn],
                 lambda e: e.scalar_tensor_tensor(out=cqn[:], in0=pq0[:], scalar=vec[:, 5:6], in1=rstd[:],
                                                  op0=ALU.mult, op1=ALU.mult))
            S.op("dve", [B_pq1, B_r, Bc], [B_cqn2],
                 lambda e: e.scalar_tensor_tensor(out=cqn2[:], in0=pq1[:], scalar=vec[:, 6:7], in1=rstd[:],
                                                  op0=ALU.mult, op1=ALU.mult))
            cq = [(cqn, B_cqn), (cqn2, B_cqn2)]
            pqn, B_pqn = pA.next()
            for c in range(2):
                S.op("pe", [cq[c][1], Bc], [B_pqn],
                     lambda e: e.matmul(pqn[:], lhsT=wuq_sb[:, c, 0:128], rhs=cq[c][0][:], start=(c == 0), stop=(c == 1)),
                     inc=(c == 1))
            S.op("act", [B_pqn], [B_q], lambda e: e.activation(out=q[:, 0, :], in_=pqn[:], func=AF.Copy, scale=SCALE))
            pqp, B_pqp = pA.next()
            for c in range(2):
                S.op("pe", [cq[c][1], Bc], [B_pqp],
                     lambda e: e.matmul(pqp[0:64, :], lhsT=wuq_sb[:, c, 128:192], rhs=cq[c][0][:], start=(c == 0),
                                        stop=(c == 1)), inc=(c == 1))
            pqr, B_pqr = pA.next()
            for c in range(2):
                S.op("pe", [cq[c][1], Bc], [B_pqr],
                     lambda e: e.matmul(pqr[0:64, :], lhsT=wuq_sb[:, c, 192:256], rhs=cq[c][0][:], start=(c == 0),
                                        stop=(c == 1)), inc=(c == 1))
            t1, B_t1 = f32r.next()
            t2, B_t2 = f32r.next()
            S.op("dve", [B_pqp, B_cs], [B_t1],
                 lambda e: e.scalar_tensor_tensor(out=t1[0:64, :], in0=pqp[0:64, :], scalar=SCALE, in1=cs[:, 0, :],
                                                  op0=ALU.mult, op1=ALU.mult))
            S.op("dve", [B_pqr, B_cs], [B_t2],
                 lambda e: e.scalar_tensor_tensor(out=t2[0:64, :], in0=pqr[0:64, :], scalar=SCALE, in1=cs[:, 1, :],
                                                  op0=ALU.mult, op1=ALU.mult))
            S.op("dve", [B_t1, B_t2], [B_q],
                 lambda e: e.tensor_tensor(out=q[0:64, 1, :], in0=t1[0:64, :], in1=t2[0:64, :], op=ALU.add))
            pkp, B_pkp = proj(hb, B_hb, 320, 64)
            pkr, B_pkr = proj(hb, B_hb, 384, 64)
            t1, B_t1 = f32r.next()
            t2, B_t2 = f32r.next()
            S.op("dve", [B_pkp, B_cs], [B_t1],
                 lambda e: e.tensor_tensor(out=t1[0:64, :], in0=pkp[0:64, :], in1=cs[:, 0, :], op=ALU.mult))
            S.op("dve", [B_pkr, B_cs], [B_t2],
                 lambda e: e.tensor_tensor(out=t2[0:64, :], in0=pkr[0:64, :], in1=cs[:, 1, :], op=ALU.mult))
            S.op("dve", [B_t1, B_t2], [B_K[t]],
                 lambda e: e.tensor_tensor(out=KpT[:, bsl], in0=t1[0:64, :], in1=t2[0:64, :], op=ALU.add))
            pkv, B_pkv = proj(hb, B_hb, 768, 128)
            rstd, B_r = rms_rstd([(pkv[:], B_pkv)], 128, 128)
            ckvn, B_ckvn = bfr.next()
            S.op("dve", [B_pkv, B_r, Bc], [B_ckvn],
                 lambda e: e.scalar_tensor_tensor(out=ckvn[:], in0=pkv[:], scalar=vec[:, 7:8], in1=rstd[:],
                                                  op0=ALU.mult, op1=ALU.mult))
            pkn, B_pkn = pA.next()
            S.op("pe", [B_ckvn, Bc], [B_pkn],
                 lambda e: e.matmul(pkn[:], lhsT=wukv_sb[:, 0:128], rhs=ckvn[:], start=True, stop=True))
            S.op("act", [B_pkn], [B_K[t]], lambda e: e.activation(out=KnT[:, bsl], in_=pkn[:], func=AF.Copy))
            pv, B_pv = pA.next()
            for s in range(4):
                S.op("pe", [B_ckvn, Bc], [B_pv],
                     lambda e: e.matmul(pv[:, s * 128:(s + 1) * 128], lhsT=ckvn[:, s * 128:(s + 1) * 128],
                                        rhs=wukv_sb[:, 128:256], start=True, stop=True), inc=(s == 3))
            S.op("act", [B_pv], [B_K[t]],
                 lambda e: e.activation(out=Vt[:, 4 * t:4 * t + 4, :],
                                        in_=pv[:].rearrange("p (s d) -> p s d", s=4), func=AF.Copy))

            phq, B_phq = proj(hb, B_hb, 0, 128)
            phf, B_phf = proj(hb, B_hb, 128, 128)
            sig, B_sig = f32r.next()
            S.op("act", [B_phf], [B_sig], lambda e: e.activation(out=sig[:], in_=phf[:], func=AF.Sigmoid))
            qf, B_qf = f32r.next()
            S.op("act", [B_phq], [B_qf], lambda e: e.activation(out=qf[:], in_=phq[:], func=AF.Silu))
            lf, B_lf = f32r.next()
            S.op("dve", [B_sig, Bc], [B_lf],
                 lambda e: e.tensor_scalar(out=lf[:], in0=sig[:], scalar1=lbv[:, 1:2], scalar2=lbv[:, 0:1],
                                           op0=ALU.mult, op1=ALU.add))
            S.op("dve", [B_lf], [B_lf], lambda e: e.tensor_scalar_max(out=lf[:], in0=lf[:], scalar1=1e-20))
            S.op("act", [B_lf], [B_lf], lambda e: e.activation(out=lf[:], in_=lf[:], func=AF.Ln))
            kk, B_kk = f32r.next()
            S.op("dve", [B_sig, Bc], [B_kk],
                 lambda e: e.tensor_scalar(out=kk[:], in0=sig[:], scalar1=lbv[:, 2:3], scalar2=lbv[:, 1:2],
                                           op0=ALU.mult, op1=ALU.add))
            bb, B_bb = f32r.next()
            S.op("dve", [B_lf, Bc], [B_bb],
                 lambda e: e.tensor_tensor_scan(out=bb[:], data0=cmask[:], data1=lf[:], initial=0.0,
                                                op0=ALU.mult, op1=ALU.add))
            b3 = bb[:].rearrange("p (c j) -> p c j", j=64)
            d1, B_d1 = f32r.next()
            d2, B_d2 = f32r.next()
            S.op("dve", [B_bb], [B_d1],
                 lambda e: e.tensor_tensor(out=d1[:].rearrange("p (c j) -> p c j", j=64), in0=b3,
                                           in1=b3[:, :, 31:32].to_broadcast([128, 8, 64]), op=ALU.subtract))
            S.op("dve", [B_bb], [B_d2],
                 lambda e: e.tensor_tensor(out=d2[:].rearrange("p (c j) -> p c j", j=64), in0=b3,
                                           in1=b3[:, :, 63:64].to_broadcast([128, 8, 64]), op=ALU.subtract))
            e1, B_e1 = f32r.next()
            S.op("act", [B_d1], [B_e1], lambda e: e.activation(out=e1[:], in_=d1[:], func=AF.Exp))
            qt, B_qt = bfr.next()
            S.op("dve", [B_qf, B_e1], [B_qt], lambda e: e.tensor_tensor(out=qt[:], in0=qf[:], in1=e1[:], op=ALU.mult))
            S.op("act", [B_d1], [B_e1], lambda e: e.activation(out=e1[:], in_=d1[:], func=AF.Exp, scale=-1.0))
            kt, B_kt = bfr.next()
            S.op("dve", [B_kk, B_e1], [B_kt], lambda e: e.tensor_tensor(out=kt[:], in0=kk[:], in1=e1[:], op=ALU.mult))
            S.op("act", [B_d2], [B_d2], lambda e: e.activation(out=d2[:], in_=d2[:], func=AF.Exp, scale=-1.0))
            kh, B_kh = bfr.next()
            S.op("dve", [B_kk, B_d2], [B_kh], lambda e: e.tensor_tensor(out=kh[:], in0=kk[:], in1=d2[:], op=ALU.mult))
            S.op("act", [B_bb], [B_bb], lambda e: e.activation(out=bb[:], in_=bb[:], func=AF.Exp))
            qh, B_qh = bfr.next()
            S.op("dve", [B_qf, B_bb], [B_qh], lambda e: e.tensor_tensor(out=qh[:], in0=qf[:], in1=bb[:], op=ALU.mult))
            pV, B_pV = pA.next()
            for c in range(8):
                csl = slice(c * 64, (c + 1) * 64)
                for k in range(8):
                    S.op("pe", [B_hb, Bc], [B_pV],
                         lambda e: e.matmul(pV[0:64, csl], lhsT=hb[:, k, csl], rhs=win[:, k, 896:960],
                                            start=(k == 0), stop=(k == 7)), inc=(k == 7 and c == 7))
            vt, B_vt = bfr.next()
            S.op("act", [B_pV], [B_vt], lambda e: e.activation(out=vt[0:64, :], in_=pV[0:64, :], func=AF.Copy))
            pAT, B_pAT = pA.next()
            for c in range(8):
                csl = slice(c * 64, (c + 1) * 64)
                S.op("pe", [B_kt, B_qt], [B_pAT],
                     lambda e: e.matmul(pAT[0:64, csl], lhsT=kt[:, csl], rhs=qt[:, csl], start=True, stop=True),
                     inc=(c == 7))
            at, B_at = bfr.next()
            S.op("dve", [B_pAT, Bc], [B_at],
                 lambda e: e.tensor_tensor(out=at[0:64, :], in0=pAT[0:64, :], in1=tri8[0:64, :], op=ALU.mult))
            pKT, B_pKT = pA.next()
            pKTb = pKT[:].bitcast(BF16)
            for c in range(8):
                csl = slice(c * 64, (c + 1) * 64)
                S.op("pe", [B_kh, Bc], [B_pKT],
                     lambda e: e.transpose(out=pKTb[0:64, c * 128:(c + 1) * 128], in_=kh[:, csl], identity=ident_bf[:]),
                     inc=(c == 7))
            kht, B_kht = khtr.next()
            S.op("act", [B_pKT], [B_kht], lambda e: e.activation(out=kht[0:64, :], in_=pKTb[0:64, :], func=AF.Copy))
            for c in range(8):
                csl = slice(c * 64, (c + 1) * 64)
                S.op("pe", [B_kht, B_vt], [B_pH],
                     lambda e: e.matmul(pH[:, csl], lhsT=kht[0:64, c * 128:(c + 1) * 128], rhs=vt[0:64, csl],
                                        start=True, stop=True), inc=(c == 7))
            pOh, B_pOh = pA.next()
            for c in range(8):
                csl = slice(c * 64, (c + 1) * 64)
                S.op("pe", [B_vt, B_at], [B_pOh],
                     lambda e: e.matmul(pOh[0:64, csl], lhsT=vt[0:64, csl], rhs=at[0:64, csl], start=True, stop=False),
                     inc=False)
                S.op("pe", [B_Sb[c % 2], B_qh], [B_pOh],
                     lambda e: e.matmul(pOh[0:64, csl], lhsT=Sbf[:, c % 2, :], rhs=qh[:, csl], start=False, stop=True))
                S.op("dve", [B_pH, B_bb, B_S], [B_S],
                     lambda e: e.scalar_tensor_tensor(out=Sst[:], in0=Sst[:], scalar=bb[:, c * 64 + 63:c * 64 + 64],
                                                      in1=pH[:, csl], op0=ALU.mult, op1=ALU.add))
                S.op("dve", [B_S], [B_Sb[(c + 1) % 2]],
                     lambda e: e.tensor_copy(out=Sbf[:, (c + 1) % 2, :], in_=Sst[:]))
            oh, B_oh = f32r.next()
            S.op("act", [B_pOh], [B_oh], lambda e: e.activation(out=oh[0:64, :], in_=pOh[0:64, :], func=AF.Copy))
            phg, B_phg = proj(hb, B_hb, 256, 64)
            sg, B_sg = f32r.next()
            S.op("act", [B_phg], [B_sg], lambda e: e.activation(out=sg[0:64, :], in_=phg[0:64, :], func=AF.Silu))
            rstd, B_r = rms_rstd([(oh[0:64, :], B_oh)], 64, 64)
            S.op("dve", [B_oh, B_r, Bc], [B_oh],
                 lambda e: e.scalar_tensor_tensor(out=oh[0:64, :], in0=oh[0:64, :], scalar=vec[0:64, 4:5],
                                                  in1=rstd[0:64, :], op0=ALU.mult, op1=ALU.mult))
            S.op("dve", [B_oh, B_sg], [B_ot],
                 lambda e: e.tensor_tensor(out=ot[0:64, 0, :], in0=oh[0:64, :], in1=sg[0:64, :], op=ALU.mult))

            ppool, B_pp = pA.next()
            for s in range(4):
                gt = 4 * t + s
                for k in range(8):
                    S.op("pe", [B_hb, Bc], [B_pH],
                         lambda e: e.matmul(pH[:, 384:448], lhsT=hb[:, k, s * 128:(s + 1) * 128], rhs=win[:, k, 960:1024],
                                            start=(k == 0), stop=(k == 7)), inc=(k == 7))
                xp, B_xp = xpr.next()
                S.op("act", [B_pH], [B_xp], lambda e: e.activation(out=xp[:], in_=pH[:, 384:448], func=AF.Copy))
                bi = 0 if gt == 0 else 1
                S.op("pe", [B_xp, Bc], [B_pp],
                     lambda e: e.matmul(ppool[0:64, s * 128:(s + 1) * 128], lhsT=xp[:], rhs=band_sb[:, bi, :],
                                        start=True, stop=(gt == 0)), inc=(gt == 0))
                if gt > 0:
                    pxp, B_pxp = prev_xp
                    S.op("pe", [B_pxp, Bc], [B_pp],
                         lambda e: e.matmul(ppool[0:64, s * 128:(s + 1) * 128], lhsT=pxp[:], rhs=band_sb[:, 2, :],
                                            start=False, stop=True))
                prev_xp = (xp, B_xp)
            pl_bf, B_plbf = bfr.next()
            S.op("act", [B_pp], [B_plbf], lambda e: e.activation(out=pl_bf[0:64, :], in_=ppool[0:64, :], func=AF.Copy))
            py, B_py = pA.next()
            S.op("pe", [B_plbf, Bc], [B_py],
                 lambda e: e.matmul(py[0:64, :], lhsT=poolw_sb[:, :], rhs=pl_bf[0:64, :], start=True, stop=True))
            S.op("act", [B_py, Bc], [B_ot],
                 lambda e: e.activation(out=ot[0:64, 2, :], in_=py[0:64, :], func=AF.Copy, scale=vec[0:64, 8:9]))

            nkb = 4 * t + 4
            for kb in range(nkb):
                j = kb - 4 * t
                q0 = 128 * j if j > 0 else 0
                nq = 512 - q0
                ksl = slice(kb * 128, (kb + 1) * 128)
                ps_, B_ps = pS.next()
                S.op("pe", [B_K[kb // 4], B_q], [B_ps],
                     lambda e: e.matmul(ps_[:, 0:nq], lhsT=KnT[:, ksl], rhs=q[:, 0, q0:512], start=True, stop=False),
                     inc=False)
                S.op("pe", [B_K[kb // 4], B_q], [B_ps],
                     lambda e: e.matmul(ps_[:, 0:nq], lhsT=KpT[:, ksl], rhs=q[0:64, 1, q0:512], start=False, stop=True))
                pt, B_pt = ptr.next()
                S.op("act", [B_ps], [B_pt], lambda e: e.activation(out=pt[:, 0:nq], in_=ps_[:, 0:nq], func=AF.Exp))
                if j >= 0:
                    S.op("dve", [B_pt, Bc], [B_pt],
                         lambda e: e.tensor_tensor(out=pt[:, 0:128], in0=pt[:, 0:128], in1=tri_bf[:], op=ALU.mult))
                S.op("pe", [B_K[kb // 4], B_pt], [B_pO],
                     lambda e: e.matmul(pO[:, q0:512], lhsT=Vt[:, kb, :], rhs=pt[:, 0:nq], start=(kb == 0),
                                        stop=(kb == nkb - 1)), inc=False)
                S.op("pe", [Bc, B_pt], [B_pO, B_pL],
                     lambda e: e.matmul(pL[:, q0:512], lhsT=ones_bf[:], rhs=pt[:, 0:nq], start=(kb == 0),
                                        stop=(kb == nkb - 1)))
            rl, B_rl = f32r.next()
            S.op("act", [B_pL], [B_rl], lambda e: e.activation(out=rl[:], in_=pL[:], func=AF.Ln))
            S.op("act", [B_rl], [B_rl], lambda e: e.activation(out=rl[:], in_=rl[:], func=AF.Exp, scale=-1.0))
            S.op("dve", [B_pO, B_rl], [B_ot],
                 lambda e: e.tensor_tensor(out=ot[:, 1, :], in0=pO[:], in1=rl[:], op=ALU.mult))
            S.dma("sp", ch_out[t % 2], [(oT[0:64, bsl], ot[0:64, 0, :]), (oT[64:192, bsl], ot[:, 1, :]),
                                        (oT[192:256, bsl], ot[0:64, 2, :])], [B_ot], [])
        S.finish("sp")
    return nc


POOL_WINDOWS = (2, 4, 8, 16)


def _gl(g):
    return np.ascontiguousarray(np.asarray(g, np.float32).reshape(8, 128).T)


def _rope_tables(ns):
    pos = np.arange(ns, dtype=np.float32)
    inv_freq = (1.0 / (np.float32(10000.0) ** (np.arange(0, 64, 2, dtype=np.float32) / np.float32(64)))).astype(np.float32)
    ang = (pos[:, None] * inv_freq[None, :]).astype(np.float32)
    cos = np.cos(ang).astype(np.float32).T
    sin = np.sin(ang).astype(np.float32).T
    cs2 = np.empty((64, 2, ns), np.float32)
    cs2[0:32, 0] = cos
    cs2[32:64, 0] = cos
    cs2[0:32, 1] = -sin
    cs2[32:64, 1] = sin
    return cs2


def _band(w):
    b = np.zeros((128, 3, 128), np.float32)
    s = np.arange(128)[:, None]
    t = np.arange(128)[None, :]
    win = (s <= t) & (s > t - w)
    eye = (s == t).astype(np.float32)
    cnt = np.minimum(t + 1, w).astype(np.float32)
    b[:, 0, :] = win / cnt - eye
    b[:, 1, :] = win / np.float32(w) - eye
    b[:, 2, :] = ((s - 128) > (t - w)).astype(np.float32) / np.float32(w)
    return b


def _consts():
    c = np.zeros((128, 3, 512), np.float32)
    c[:, 0, 0:128] = np.eye(128, dtype=np.float32)
    k = np.arange(128)[:, None]
    q = np.arange(128)[None, :]
    c[:, 1, 0:128] = (q >= k).astype(np.float32)
    c[:, 2, :] = 1.0
    c[:, 2, 0::64] = 0.0
    return c


def prep_B(inp, l, r, ns):
    w_in = np.asarray(inp["w_in"][l], np.float32)
    winh = np.zeros((D, 1024), np.float32)
    winh[:, 0:128] = w_in[:, r * 128:(r + 1) * 128]
    winh[:, 128:256] = w_in[:, 512 + r * 128:512 + (r + 1) * 128]
    winh[:, 256:320] = w_in[:, 1280 + r * 64:1280 + (r + 1) * 64]
    kpe = w_in[:, 1920:1984]
    winh[:, 320:384] = kpe
    winh[:, 384:416] = kpe[:, 32:64]
    winh[:, 416:448] = kpe[:, 0:32]
    winh[:, 512:768] = w_in[:, 1536:1792]
    winh[:, 768:896] = w_in[:, 1792:1920]
    winh[:, 896:960] = w_in[:, 1024 + r * 64:1024 + (r + 1) * 64]
    winh[:, 960:1024] = w_in[:, 1984 + r * 64:1984 + (r + 1) * 64]
    uq = np.asarray(inp["mla_w_uq"][l], np.float32)[:, r * 192:(r + 1) * 192]
    wuq = np.zeros((256, 256), np.float32)
    wuq[:, 0:192] = uq
    wuq[:, 192:224] = uq[:, 160:192]
    wuq[:, 224:256] = uq[:, 128:160]
    wukv = np.ascontiguousarray(np.asarray(inp["mla_w_ukv"][l], np.float32)[:, r * 256:(r + 1) * 256])
    poolw = np.ascontiguousarray(np.asarray(inp["pool_w"][l, r], np.float32))
    vecs = np.zeros((128, 16), np.float32)
    lbr = np.asarray(inp["hgrn_lower_bounds"], np.float32)
    vecs[:, 1] = lbr[0, r * 128:(r + 1) * 128]
    vecs[:, 2] = lbr[1, r * 128:(r + 1) * 128]
    vecs[:, 3] = 1.0 if l == 1 else 0.0
    vecs[0:64, 4] = np.asarray(inp["hgrn_out_norm"][l], np.float32)[r * 64:(r + 1) * 64]
    vecs[:, 5] = np.asarray(inp["mla_q_norm"][l], np.float32)[0:128]
    vecs[:, 6] = np.asarray(inp["mla_q_norm"][l], np.float32)[128:256]
    vecs[:, 7] = np.asarray(inp["mla_kv_norm"][l], np.float32)
    vecs[0:64, 8] = np.asarray(inp["pool_scale"][l], np.float32)[r * 64:(r + 1) * 64]
    return dict(winh=winh, wuq=wuq, wukv=wukv, poolw=poolw, vecs=vecs, cs2=_rope_tables(ns),
                band=_band(POOL_WINDOWS[r]), consts=_consts())


_PROGS = {}


def _prog(key, fn):
    if key not in _PROGS:
        _PROGS[key] = fn()
    return _PROGS[key]


def _wo_perm(w_o):
    idx = []
    for r in range(4):
        idx += list(range(r * 64, (r + 1) * 64))
        idx += list(range(256 + r * 128, 256 + (r + 1) * 128))
        idx += list(range(768 + r * 64, 768 + (r + 1) * 64))
    return np.ascontiguousarray(np.asarray(w_o, np.float32)[np.asarray(idx)])


def _run(nc, in_maps):
    res = run_bass_kernel_spmd(nc, in_maps, core_ids=list(range(NCORES)))
    return res.results


def kernel(**inp):
    inp = {k: np.asarray(v) for k, v in inp.items()}
    x = np.asarray(inp["x"], np.float32).reshape(BATCH * SEQ, D)
    ident = np.eye(128, dtype=np.float32)
    ncA = _prog("A", build_A)
    gv = np.concatenate([_gl(inp["attn_norm"][0]), _gl(inp["attn_norm"][0])], axis=1)
    xTs = [np.ascontiguousarray(x[c * TOK:(c + 1) * TOK].T) for c in range(NCORES)]
    rA = _run(ncA, [dict(xT=xTs[c], gvec=gv) for c in range(NCORES)])
    hTs = [rA[c]["hT"] for c in range(NCORES)]
    out = None
    for l in range(2):
        ncB = _prog("B", build_B)
        hfull = [np.ascontiguousarray(np.concatenate(hTs[4 * b:4 * b + 4], axis=1)) for b in range(BATCH)]
        insB = []
        for c in range(NCORES):
            d = prep_B(inp, l, c % 4, SEQ)
            d["hT"] = hfull[c // 4]
            insB.append(d)
        rB = _run(ncB, insB)
        wo = _wo_perm(inp["w_o"][l])
        insC = []
        for c in range(NCORES):
            b, qd = c // 4, c % 4
            oT = np.ascontiguousarray(np.concatenate(
                [rB[4 * b + r]["oTo"][:, qd * TOK:(qd + 1) * TOK] for r in range(4)], axis=0))
            d = dict(xT=xTs[c], oT=oT, wo=wo, ident=ident)
            if l == 0:
                d["gvec"] = np.concatenate([_gl(inp["ffn_norm"][0]), _gl(inp["attn_norm"][1])], axis=1)
                d["wg"] = np.asarray(inp["dense_w_gate"][0], np.float32)
                d["wu"] = np.asarray(inp["dense_w_up"][0], np.float32)
                d["wd"] = np.asarray(inp["dense_w_down"][0], np.float32)
            else:
                d["gvec"] = np.concatenate([_gl(inp["ffn_norm"][1]), _gl(inp["final_norm"])], axis=1)
                d["wg"] = np.asarray(inp["moe_w_gate"][0], np.float32)
                d["wu"] = np.asarray(inp["moe_w_up"][0], np.float32)
                d["wd"] = np.asarray(inp["moe_w_down"][0], np.float32)
                d["router"] = np.asarray(inp["moe_router"][0], np.float32)
            insC.append(d)
        if l == 0:
            rC = _run(_prog("C0", lambda: build_C("dense", False)), insC)
            xTs = [rC[c]["x2T"] for c in range(NCORES)]
            hTs = [rC[c]["hnT"] for c in range(NCORES)]
        else:
            rC = _run(_prog("C1", lambda: build_C("moe", True)), insC)
            out = np.concatenate([rC[c]["outT"].T for c in range(NCORES)], axis=0)
    return np.ascontiguousarray(out.reshape(BATCH, SEQ, D).astype(np.float32))
```

```python
import numpy as np
from contextlib import ExitStack
import ml_dtypes
import concourse.bass as bass
import concourse.mybir as mybir
from concourse.bass_utils import run_bass_kernel_spmd

F32 = mybir.dt.float32
BF16 = mybir.dt.bfloat16
AF = mybir.ActivationFunctionType
ALU = mybir.AluOpType
AX = mybir.AxisListType

NCORES = 8
D = 1024
SEQ = 16384
BATCH = 2
TOK = 4096
DFF = 2560
DFFE = 3584
NEXP = 8
EPS = 1e-6
D_IN = 2240


class Buf:
    __slots__ = ("name", "w", "r")

    def __init__(self, name=""):
        self.name = name
        self.w = None
        self.r = []


class Chan:
    __slots__ = ("sem", "total", "id")


class Sync:
    def __init__(self, nc, stack):
        self.nc = nc
        self.stack = stack
        self.eng = {"pe": nc.tensor, "act": nc.scalar, "dve": nc.vector,
                    "pool": nc.gpsimd, "sp": nc.sync}
        self.sem = {}
        self.cnt = {}
        self.seen = {}
        for k in self.eng:
            self.sem[k] = stack.enter_context(nc.semaphore("s_" + k))
            self.cnt[k] = 0
            self.seen[k] = {}
        self.chans = []

    def chan(self, name=""):
        c = Chan()
        c.sem = self.stack.enter_context(self.nc.semaphore("d%d_%s" % (len(self.chans), name)))
        c.total = 0
        c.id = len(self.chans)
        self.chans.append(c)
        return c

    def _wait(self, e, ev):
        kind, key, val = ev
        if kind == "e" and key == e and e == "pe":
            return
        k = (kind, key if kind == "e" else key.id)
        if self.seen[e].get(k, 0) >= val:
            return
        self.seen[e][k] = val
        sem = self.sem[key] if kind == "e" else key.sem
        self.eng[e].wait_ge(sem, val)

    def _deps(self, e, reads, writes):
        for b in reads:
            if b.w is not None:
                self._wait(e, b.w)
        for b in writes:
            if b.w is not None:
                self._wait(e, b.w)
            for ev in b.r:
                if ev[0] == "e" and ev[1] == e:
                    continue
                self._wait(e, ev)

    def _mark(self, ev, reads, writes):
        for b in writes:
            b.w = ev
            b.r = []
        for b in reads:
            if b in writes:
                continue
            b.r = [x for x in b.r if not (x[0] == ev[0] and x[1] is ev[1])]
            b.r.append(ev)

    def op(self, e, reads, writes, fn, inc=True):
        self._deps(e, reads, writes)
        ins = fn(self.eng[e])
        if inc:
            ins.then_inc(self.sem[e], 1)
            self.cnt[e] += 1
            ev = ("e", e, self.cnt[e])
        else:
            ev = ("e", e, self.cnt[e] + 1)
        self._mark(ev, reads, writes)
        return ins

    def dma(self, q, ch, pairs, reads, writes, **kw):
        self._deps(q, reads, writes)
        for (o, i) in pairs:
            ins = self.eng[q].dma_start(out=o, in_=i, **kw)
            ins.then_inc(ch.sem, 16)
            ch.total += 16
        ev = ("d", ch, ch.total)
        self._mark(ev, reads, writes)

    def barrier(self):
        for e in self.eng:
            for f in self.eng:
                if f != e and self.cnt[f] > 0:
                    self._wait(e, ("e", f, self.cnt[f]))
            for c in self.chans:
                if c.total > 0:
                    self._wait(e, ("d", c, c.total))

    def finish(self, e="sp"):
        for c in self.chans:
            if c.total > 0:
                self._wait(e, ("d", c, c.total))
        for f in self.eng:
            if f != e and self.cnt[f] > 0:
                self._wait(e, ("e", f, self.cnt[f]))


class Ring:
    def __init__(self, items):
        self.items = items
        self.i = 0

    def next(self):
        it = self.items[self.i % len(self.items)]
        self.i += 1
        return it


class Ctx:
    def __init__(self, nc, stack):
        self.nc = nc
        self.st = stack
        self.S = Sync(nc, stack)
        self.n = 0

    def sb(self, shape, dt, name=None):
        self.n += 1
        t = self.st.enter_context(self.nc.sbuf_tensor("S_" + (name or ("sb%d" % self.n)), list(shape), dt))
        return t

    def ps(self, shape, dt=F32, name=None):
        self.n += 1
        t = self.st.enter_context(self.nc.psum_tensor("P_" + (name or ("ps%d" % self.n)), list(shape), dt))
        return t

    def sb_ring(self, n, shape, dt, name):
        return Ring([(self.sb(shape, dt, "%s%d" % (name, i)), Buf("%s%d" % (name, i))) for i in range(n)])

    def ps_ring(self, n, shape, dt, name):
        return Ring([(self.ps(shape, dt, "%s%d" % (name, i)), Buf("%s%d" % (name, i))) for i in range(n)])


def emit_rmsnorm(C, K, xs, B_xs, g_sb, out_fn, NT):
    S = C.S
    for tt in range(NT // 512):
        sl = slice(tt * 512, (tt + 1) * 512)
        sq, B_sq = K["sq"].next()
        for oc in range(8):
            S.op("act", [B_xs[oc][tt]], [B_sq],
                 lambda e: e.activation(out=sq[:, oc, :], in_=xs[:, oc, sl], func=AF.Square))
        pss, B_pss = K["pmisc"].next()
        for oc in range(8):
            S.op("pe", [B_sq, K["B_const"]], [B_pss],
                 lambda e: e.matmul(pss[:], lhsT=K["ones_bf"][:], rhs=sq[:, oc, :],
                                    start=(oc == 0), stop=(oc == 7)), inc=(oc == 7))
        rstd, B_rstd = K["rstd"].next()
        S.op("act", [B_pss, K["B_const"]], [B_rstd],
             lambda e: e.activation(out=rstd[:], in_=pss[:], func=AF.Ln, scale=1.0 / D,
                                    bias=K["eps"][:, 0:1]))
        S.op("act", [B_rstd], [B_rstd],
             lambda e: e.activation(out=rstd[:], in_=rstd[:], func=AF.Exp, scale=-0.5))
        for oc in range(8):
            o_ap, wb = out_fn(oc, tt)
            S.op("dve", [B_xs[oc][tt], B_rstd, K["B_const"]], wb,
                 lambda e: e.scalar_tensor_tensor(out=o_ap, in0=xs[:, oc, sl], scalar=g_sb[:, oc:oc + 1],
                                                  in1=rstd[:], op0=ALU.mult, op1=ALU.mult))


def emit_down(C, K, src, B_src, nf, wd, xs, B_xs, NT, bc=None, B_bc=None):
    S = C.S
    for oc in range(8):
        dsl, B_d = K["dslot"].next()
        S.dma("pool", K["ch_d"][(K["dslot"].i - 1) % len(K["ch_d"])],
              [(dsl[:, 0:nf, :], wd[:, oc * 128:(oc + 1) * 128].rearrange("(f p) n -> p f n", p=128))],
              [], [B_d])
        for tt in range(NT // 512):
            sl = slice(tt * 512, (tt + 1) * 512)
            py, B_py = K["py"].next()
            for f in range(nf):
                S.op("pe", [B_d, B_src[tt]], [B_py],
                     lambda e: e.matmul(py[:], lhsT=dsl[:, f, :], rhs=src[:, f, sl],
                                        start=(f == 0), stop=(f == nf - 1)), inc=(f == nf - 1))
            if bc is None:
                S.op("dve", [B_py, B_xs[oc][tt]], [B_xs[oc][tt]],
                     lambda e: e.tensor_tensor(out=xs[:, oc, sl], in0=py[:], in1=xs[:, oc, sl], op=ALU.add))
            else:
                tmp, B_tmp = K["sil"].next()
                S.op("dve", [B_py, B_bc], [B_tmp],
                     lambda e: e.tensor_tensor(out=tmp[:], in0=py[:], in1=bc[:, sl], op=ALU.mult))
                S.op("dve", [B_tmp, B_xs[oc][tt]], [B_xs[oc][tt]],
                     lambda e: e.tensor_tensor(out=xs[:, oc, sl], in0=tmp[:], in1=xs[:, oc, sl], op=ALU.add))


def emit_gate_up(C, K, h2, B_h2, wg, wu, nf, a, B_a, NT):
    S = C.S
    for f in range(nf):
        gu, B_gu = K["guslot"].next()
        S.dma("pool", K["ch_gu"][(K["guslot"].i - 1) % len(K["ch_gu"])],
              [(gu[:, :, 0:128], wg[:, f * 128:(f + 1) * 128].rearrange("(k p) n -> p k n", p=128)),
               (gu[:, :, 128:256], wu[:, f * 128:(f + 1) * 128].rearrange("(k p) n -> p k n", p=128))],
              [], [B_gu])
        for tt in range(NT // 512):
            sl = slice(tt * 512, (tt + 1) * 512)
            pg, B_pg = K["pg"].next()
            pu, B_pu = K["pu"].next()
            for k in range(8):
                S.op("pe", [B_gu, B_h2[tt]], [B_pg],
                     lambda e: e.matmul(pg[:], lhsT=gu[:, k, 0:128], rhs=h2[:, k, sl],
                                        start=(k == 0), stop=(k == 7)), inc=(k == 7))
            for k in range(8):
                S.op("pe", [B_gu, B_h2[tt]], [B_pu],
                     lambda e: e.matmul(pu[:], lhsT=gu[:, k, 128:256], rhs=h2[:, k, sl],
                                        start=(k == 0), stop=(k == 7)), inc=(k == 7))
            sil, B_sil = K["sil"].next()
            S.op("act", [B_pg], [B_sil],
                 lambda e: e.activation(out=sil[:], in_=pg[:], func=AF.Silu))
            S.op("dve", [B_sil, B_pu], [B_a[tt]],
                 lambda e: e.tensor_tensor(out=a[:, f, sl], in0=pu[:], in1=sil[:], op=ALU.mult))


def emit_router(C, K, h2, B_h2, rt_sb, combT, B_combT, NT):
    S = C.S
    nc = C.nc
    for s in range(NT // 128):
        tt = s // 4
        tsl = slice(s * 128, (s + 1) * 128)
        pl, B_pl = K["pmisc"].next()
        for k in range(8):
            S.op("pe", [B_h2[tt], K["B_const"]], [B_pl],
                 lambda e: e.matmul(pl[:, 0:8], lhsT=h2[:, k, tsl], rhs=rt_sb[:, k, :],
                                    start=(k == 0), stop=(k == 7)), inc=(k == 7))
        r, B_r = K["rt"].next()
        S.op("act", [B_pl], [B_r], lambda e: e.activation(out=r[:, 0:8], in_=pl[:, 0:8], func=AF.Copy))
        S.op("dve", [B_r], [B_r], lambda e: e.max(out=r[:, 8:16], in_=r[:, 0:8]))
        S.op("dve", [B_r], [B_r], lambda e: e.tensor_scalar(out=r[:, 16:24], in0=r[:, 0:8], scalar1=r[:, 9:10],
                                                            scalar2=None, op0=ALU.is_ge))
        S.op("dve", [B_r], [B_r], lambda e: e.tensor_scalar(out=r[:, 32:33], in0=r[:, 8:9], scalar1=-1.0,
                                                            scalar2=None, op0=ALU.mult))
        S.op("act", [B_r], [B_r], lambda e: e.activation(out=r[:, 24:32], in_=r[:, 0:8], func=AF.Exp,
                                                         bias=r[:, 32:33], scale=1.0))
        S.op("dve", [B_r], [B_r], lambda e: e.tensor_tensor(out=r[:, 24:32], in0=r[:, 24:32], in1=r[:, 16:24],
                                                            op=ALU.mult))
        S.op("dve", [B_r], [B_r], lambda e: e.reduce_sum(out=r[:, 33:34], in_=r[:, 24:32], axis=AX.X))
        S.op("dve", [B_r], [B_r], lambda e: e.reciprocal(out=r[:, 34:35], in_=r[:, 33:34]))
        S.op("dve", [B_r], [B_r], lambda e: e.tensor_scalar(out=r[:, 40:48], in0=r[:, 24:32], scalar1=r[:, 34:35],
                                                            scalar2=None, op0=ALU.mult))
        pt, B_pt = K["pmisc"].next()
        S.op("pe", [B_r, K["B_const"]], [B_pt],
             lambda e: e.transpose(out=pt[0:8, 0:128], in_=r[:, 40:48], identity=K["ident"][:]))
        S.op("act", [B_pt], [B_combT],
             lambda e: e.activation(out=combT[0:8, tsl], in_=pt[0:8, 0:128], func=AF.Copy))


def build_C(kind, final, NT=1024):
    nc = bass.Bass("TRN2", target_bir_lowering=False)
    dt = nc.dram_tensor
    xT = dt("xT", [D, TOK], F32, kind="ExternalInput").ap()
    oT = dt("oT", [D, TOK], BF16, kind="ExternalInput").ap()
    wo = dt("wo", [D, D], F32, kind="ExternalInput").ap()
    gvec = dt("gvec", [128, 16], F32, kind="ExternalInput").ap()
    ident_d = dt("ident", [128, 128], F32, kind="ExternalInput").ap()
    if kind == "dense":
        wg = dt("wg", [D, DFF], F32, kind="ExternalInput").ap()
        wu = dt("wu", [D, DFF], F32, kind="ExternalInput").ap()
        wd = dt("wd", [DFF, D], F32, kind="ExternalInput").ap()
        nfmax = DFF // 128
    else:
        wg = dt("wg", [NEXP, D, DFFE], F32, kind="ExternalInput").ap()
        wu = dt("wu", [NEXP, D, DFFE], F32, kind="ExternalInput").ap()
        wd = dt("wd", [NEXP, DFFE, D], F32, kind="ExternalInput").ap()
        rt = dt("router", [D, NEXP], F32, kind="ExternalInput").ap()
        nfmax = DFFE // 128
    if final:
        outT = dt("outT", [D, TOK], F32, kind="ExternalOutput").ap()
    else:
        x2T = dt("x2T", [D, TOK], F32, kind="ExternalOutput").ap()
        hnT = dt("hnT", [D, TOK], BF16, kind="ExternalOutput").ap()

    with ExitStack() as st:
        C = Ctx(nc, st)
        S = C.S
        TT = NT // 512
        K = {}
        K["B_const"] = Buf("const")
        K["ones_bf"] = C.sb([128, 128], BF16, "ones_bf")
        K["eps"] = C.sb([128, 1], F32, "eps")
        K["ident"] = C.sb([128, 128], F32, "ident")
        g_sb = C.sb([128, 16], F32, "g_sb")
        ch_c = S.chan("const")
        S.op("dve", [], [K["B_const"]], lambda e: e.memset(K["ones_bf"][:], 1.0))
        S.op("dve", [], [K["B_const"]], lambda e: e.memset(K["eps"][:], EPS))
        S.dma("sp", ch_c, [(g_sb[:], gvec), (K["ident"][:], ident_d)], [], [K["B_const"]])
        if kind == "moe":
            rt_sb = C.sb([128, 8, 8], BF16, "rt_sb")
            S.dma("pool", ch_c, [(rt_sb[:], rt.rearrange("(k p) e -> p k e", p=128))], [], [K["B_const"]])
            esel = C.sb([8, 8, 128], F32, "esel")
            for e_ in range(8):
                S.op("dve", [K["B_const"]], [K["B_const"]],
                     lambda e: e.tensor_copy(out=esel[0:8, e_, :],
                                             in_=K["ident"][0:8, e_:e_ + 1].to_broadcast([8, 128])))
            combT = C.sb([8, NT], F32, "combT")
            B_combT = Buf("combT")
            bc = C.sb([128, NT], F32, "bc")
            B_bc = Buf("bc")
            K["rt"] = C.sb_ring(2, [128, 48], F32, "rts")
        xs = C.sb([128, 8, NT], F32, "xs")
        B_xs = [[Buf("xs%d_%d" % (oc, tt)) for tt in range(TT)] for oc in range(8)]
        osb = C.sb([128, 8, NT], BF16, "osb")
        B_os = [Buf("os%d" % tt) for tt in range(TT)]
        h2 = C.sb([128, 8, NT], BF16, "h2")
        B_h2 = [Buf("h2_%d" % tt) for tt in range(TT)]
        a = C.sb([128, nfmax, NT], BF16, "a")
        B_a = [Buf("a%d" % tt) for tt in range(TT)]
        K["sq"] = C.sb_ring(2, [128, 8, 512], BF16, "sq")
        K["rstd"] = C.sb_ring(2, [128, 512], F32, "rstd")
        K["sil"] = C.sb_ring(3, [128, 512], F32, "sil")
        K["guslot"] = C.sb_ring(5, [128, 8, 256], BF16, "gu")
        K["ch_gu"] = [S.chan("gu%d" % i) for i in range(5)]
        K["dslot"] = C.sb_ring(3, [128, nfmax, 128], BF16, "dsl")
        K["ch_d"] = [S.chan("d%d" % i) for i in range(3)]
        K["pg"] = C.ps_ring(2, [128, 512], F32, "pg")
        K["pu"] = C.ps_ring(2, [128, 512], F32, "pu")
        K["py"] = C.ps_ring(2, [128, 512], F32, "py")
        K["pmisc"] = C.ps_ring(2, [128, 512], F32, "pm")
        ch_x = S.chan("x")
        ch_o = S.chan("o")
        ch_out = S.chan("out")
        if final:
            ostage = C.sb_ring(2, [128, 512], F32, "ost")
        else:
            hn = C.sb([128, 8, NT], BF16, "hn")
            B_hn = [Buf("hn%d" % tt) for tt in range(TT)]

        for sti in range(TOK // NT):
            t0 = sti * NT
            S.dma("sp", ch_x, [(xs[:, :, tt * 512:(tt + 1) * 512],
                                xT[:, t0 + tt * 512:t0 + (tt + 1) * 512].rearrange("(k p) t -> p k t", p=128))
                               for tt in range(TT)],
                  [], [B_xs[oc][tt] for oc in range(8) for tt in range(TT)])
            S.dma("sp", ch_o, [(osb[:, :, tt * 512:(tt + 1) * 512],
                                oT[:, t0 + tt * 512:t0 + (tt + 1) * 512].rearrange("(k p) t -> p k t", p=128))
                               for tt in range(TT)],
                  [], B_os)
            emit_down(C, K, osb, B_os, 8, wo, xs, B_xs, NT)
            emit_rmsnorm(C, K, xs, B_xs, g_sb, lambda oc, tt: (h2[:, oc, tt * 512:(tt + 1) * 512], [B_h2[tt]]), NT)
            if kind == "dense":
                emit_gate_up(C, K, h2, B_h2, wg, wu, DFF // 128, a, B_a, NT)
                emit_down(C, K, a, B_a, DFF // 128, wd, xs, B_xs, NT)
            else:
                emit_router(C, K, h2, B_h2, rt_sb, combT, B_combT, NT)
                for ex in range(NEXP):
                    for tt in range(TT):
                        sl = slice(tt * 512, (tt + 1) * 512)
                        pb, B_pb = K["pmisc"].next()
                        S.op("pe", [B_combT, K["B_const"]], [B_pb],
                             lambda e: e.matmul(pb[:], lhsT=esel[0:8, ex, :], rhs=combT[0:8, sl],
                                                start=True, stop=True))
                        S.op("act", [B_pb], [B_bc],
                             lambda e: e.activation(out=bc[:, sl], in_=pb[:], func=AF.Copy))
                    emit_gate_up(C, K, h2, B_h2, wg[ex], wu[ex], DFFE // 128, a, B_a, NT)
                    emit_down(C, K, a, B_a, DFFE // 128, wd[ex], xs, B_xs, NT, bc=bc, B_bc=B_bc)
            if final:
                def out_fn(oc, tt):
                    return None
                stage_list = {}

                def fin_out(oc, tt):
                    t_, b_ = ostage.next()
                    stage_list[(oc, tt)] = (t_, b_)
                    return (t_[:], [b_])
                _emit_final(C, K, xs, B_xs, g_sb, ostage, outT, t0, ch_out, NT)
            else:
                S.dma("sp", ch_out, [(x2T[:, t0 + tt * 512:t0 + (tt + 1) * 512].rearrange("(k p) t -> p k t", p=128),
                                      xs[:, :, tt * 512:(tt + 1) * 512]) for tt in range(TT)],
                      [B_xs[oc][tt] for oc in range(8) for tt in range(TT)], [])
                emit_rmsnorm(C, K, xs, B_xs, g_sb[:, 8:16],
                             lambda oc, tt: (hn[:, oc, tt * 512:(tt + 1) * 512], [B_hn[tt]]), NT)
                S.dma("sp", ch_out, [(hnT[:, t0 + tt * 512:t0 + (tt + 1) * 512].rearrange("(k p) t -> p k t", p=128),
                                      hn[:, :, tt * 512:(tt + 1) * 512]) for tt in range(TT)],
                      B_hn, [])
        S.finish("sp")
    return nc


def _emit_final(C, K, xs, B_xs, g_sb, ostage, outT, t0, ch_out, NT):
    S = C.S
    for tt in range(NT // 512):
        sl = slice(tt * 512, (tt + 1) * 512)
        sq, B_sq = K["sq"].next()
        for oc in range(8):
            S.op("act", [B_xs[oc][tt]], [B_sq],
                 lambda e: e.activation(out=sq[:, oc, :], in_=xs[:, oc, sl], func=AF.Square))
        pss, B_pss = K["pmisc"].next()
        for oc in range(8):
            S.op("pe", [B_sq, K["B_const"]], [B_pss],
                 lambda e: e.matmul(pss[:], lhsT=K["ones_bf"][:], rhs=sq[:, oc, :],
                                    start=(oc == 0), stop=(oc == 7)), inc=(oc == 7))
        rstd, B_rstd = K["rstd"].next()
        S.op("act", [B_pss, K["B_const"]], [B_rstd],
             lambda e: e.activation(out=rstd[:], in_=pss[:], func=AF.Ln, scale=1.0 / D, bias=K["eps"][:, 0:1]))
        S.op("act", [B_rstd], [B_rstd],
             lambda e: e.activation(out=rstd[:], in_=rstd[:], func=AF.Exp, scale=-0.5))
        for oc in range(8):
            ot, B_ot = ostage.next()
            S.op("dve", [B_xs[oc][tt], B_rstd, K["B_const"]], [B_ot],
                 lambda e: e.scalar_tensor_tensor(out=ot[:], in0=xs[:, oc, sl], scalar=g_sb[:, 8 + oc:9 + oc],
                                                  in1=rstd[:], op0=ALU.mult, op1=ALU.mult))
            S.dma("sp", ch_out, [(outT[oc * 128:(oc + 1) * 128, t0 + tt * 512:t0 + (tt + 1) * 512], ot[:])],
                  [B_ot], [])


def build_A():
    nc = bass.Bass("TRN2", target_bir_lowering=False)
    xT = nc.dram_tensor("xT", [D, TOK], F32, kind="ExternalInput").ap()
    gvec = nc.dram_tensor("gvec", [128, 16], F32, kind="ExternalInput").ap()
    hT = nc.dram_tensor("hT", [D, TOK], BF16, kind="ExternalOutput").ap()
    with ExitStack() as st:
        C = Ctx(nc, st)
        S = C.S
        K = {}
        K["B_const"] = Buf("const")
        K["ones_bf"] = C.sb([128, 128], BF16, "ones_bf")
        K["eps"] = C.sb([128, 1], F32, "eps")
        g_sb = C.sb([128, 16], F32, "g_sb")
        ch_c = S.chan("const")
        S.op("dve", [], [K["B_const"]], lambda e: e.memset(K["ones_bf"][:], 1.0))
        S.op("dve", [], [K["B_const"]], lambda e: e.memset(K["eps"][:], EPS))
        S.dma("sp", ch_c, [(g_sb[:], gvec)], [], [K["B_const"]])
        K["sq"] = C.sb_ring(2, [128, 8, 512], BF16, "sq")
        K["rstd"] = C.sb_ring(2, [128, 512], F32, "rstd")
        K["pmisc"] = C.ps_ring(2, [128, 512], F32, "pm")
        xr = C.sb_ring(2, [128, 8, 512], F32, "xs")
        hr = C.sb_ring(2, [128, 8, 512], BF16, "hs")
        chx = [S.chan("x0"), S.chan("x1")]
        cho = [S.chan("o0"), S.chan("o1")]
        for j in range(TOK // 512):
            xs, B = xr.next()
            hs, Bh = hr.next()
            S.dma("sp", chx[j % 2], [(xs[:], xT[:, j * 512:(j + 1) * 512].rearrange("(k p) t -> p k t", p=128))],
                  [], [B])
            B_xs = [[B] for _ in range(8)]
            emit_rmsnorm(C, K, xs, B_xs, g_sb, lambda oc, tt: (hs[:, oc, :], [Bh]), 512)
            S.dma("sp", cho[j % 2], [(hT[:, j * 512:(j + 1) * 512].rearrange("(k p) t -> p k t", p=128), hs[:])],
                  [Bh], [])
        S.finish("sp")
    return nc


SCALE = (128 + 64) ** -0.5


def build_B(NBLK=32):
    NS = NBLK * 512
    nc = bass.Bass("TRN2", target_bir_lowering=False)
    dt = nc.dram_tensor
    hT = dt("hT", [D, NS], BF16, kind="ExternalInput").ap()
    winh = dt("winh", [D, 1024], F32, kind="ExternalInput").ap()
    wuq = dt("wuq", [256, 256], F32, kind="ExternalInput").ap()
    wukv = dt("wukv", [128, 256], F32, kind="ExternalInput").ap()
    poolw = dt("poolw", [64, 64], F32, kind="ExternalInput").ap()
    vecs = dt("vecs", [128, 16], F32, kind="ExternalInput").ap()
    cs2 = dt("cs2", [64, 2, NS], F32, kind="ExternalInput").ap()
    band = dt("band", [128, 3, 128], F32, kind="ExternalInput").ap()
    consts = dt("consts", [128, 3, 512], F32, kind="ExternalInput").ap()
    oT = dt("oTo", [256, NS], BF16, kind="ExternalOutput").ap()

    with ExitStack() as st:
        C = Ctx(nc, st)
        S = C.S
        Bc = Buf("const")
        ch_c = S.chan("const")
        pass
        win = C.sb([128, 8, 1024], BF16, "win")
        S.dma("pool", ch_c, [(win[:, k, :], winh[k * 128:(k + 1) * 128, :]) for k in range(8)], [], [Bc])
        wuq_sb = C.sb([128, 2, 256], BF16, "wuq")
        S.dma("pool", ch_c, [(wuq_sb[:], wuq.rearrange("(k p) n -> p k n", p=128))], [], [Bc])
        wukv_sb = C.sb([128, 256], BF16, "wukv")
        poolw_sb = C.sb([64, 64], BF16, "poolw")
        band_sb = C.sb([128, 3, 128], BF16, "band")
        S.dma("pool", ch_c, [(wukv_sb[:], wukv), (poolw_sb[:], poolw), (band_sb[:], band)], [], [Bc])
        vec = C.sb([128, 16], F32, "vec")
        cst = C.sb([128, 3, 512], F32, "cst")
        S.dma("sp", ch_c, [(vec[:], vecs), (cst[:], consts)], [], [Bc])
        ident_bf = C.sb([128, 128], BF16, "identbf")
        tri_bf = C.sb([128, 128], BF16, "tribf")
        cmask = C.sb([128, 512], F32, "cmask")
        ones_bf = C.sb([128, 128], BF16, "onesbf")
        eps = C.sb([128, 1], F32, "eps")
        lbv = C.sb([128, 4], F32, "lbv")
        S.op("dve", [Bc], [Bc], lambda e: e.tensor_copy(out=ident_bf[:], in_=cst[:, 0, 0:128]))
        S.op("dve", [Bc], [Bc], lambda e: e.tensor_copy(out=tri_bf[:], in_=cst[:, 1, 0:128]))
        S.op("dve", [Bc], [Bc], lambda e: e.tensor_copy(out=cmask[:], in_=cst[:, 2, :]))
        S.op("dve", [], [Bc], lambda e: e.memset(ones_bf[:], 1.0))
        S.op("dve", [], [Bc], lambda e: e.memset(eps[:], EPS))
        mhalf = C.sb([128, 512], F32, "mhalf")
        S.op("dve", [], [Bc], lambda e: e.memset(mhalf[:], -0.5))
        S.op("dve", [Bc], [Bc], lambda e: e.tensor_tensor(out=lbv[:, 3:4], in0=vec[:, 2:3], in1=vec[:, 1:2],
                                                         op=ALU.subtract))
        S.op("act", [Bc], [Bc], lambda e: e.activation(out=lbv[:, 3:4], in_=lbv[:, 3:4], func=AF.Sigmoid))
        S.op("dve", [Bc], [Bc], lambda e: e.tensor_tensor(out=lbv[:, 0:1], in0=lbv[:, 3:4], in1=vec[:, 3:4],
                                                         op=ALU.mult))
        S.op("dve", [Bc], [Bc], lambda e: e.tensor_scalar(out=lbv[:, 1:2], in0=lbv[:, 0:1], scalar1=-1.0, scalar2=1.0,
                                                         op0=ALU.mult, op1=ALU.add))
        S.op("dve", [Bc], [Bc], lambda e: e.tensor_scalar(out=lbv[:, 2:3], in0=lbv[:, 1:2], scalar1=-1.0, scalar2=None,
                                                         op0=ALU.mult))
        KnT = C.sb([128, NS], BF16, "KnT")
        KpT = C.sb([64, NS], BF16, "KpT")
        Vt = C.sb([128, NS // 128, 128], BF16, "Vt")
        B_K = [Buf("K%d" % t) for t in range(NBLK)]
        Sst = C.sb([128, 64], F32, "Sst")
        Sbf = C.sb([128, 2, 64], BF16, "Sbf")
        B_S = Buf("state")
        B_Sb = [Buf("sb0"), Buf("sb1")]
        S.op("dve", [], [B_S], lambda e: e.memset(Sst[:], 0.0))
        S.op("dve", [], B_Sb, lambda e: e.memset(Sbf[:], 0.0))
        tri8 = C.sb([64, 512], BF16, "tri8")
        for c_ in range(8):
            S.op("dve", [Bc], [Bc], lambda e: e.tensor_copy(out=tri8[:, c_ * 64:(c_ + 1) * 64], in_=cst[0:64, 1, 0:64]))
        khtr = C.sb_ring(2, [64, 1024], BF16, "kht")
        hring = C.sb_ring(2, [128, 8, 512], BF16, "hblk")
        ch_h = [S.chan("h0"), S.chan("h1")]
        csring = C.sb_ring(2, [64, 2, 512], F32, "cs")
        ch_cs = [S.chan("cs0"), S.chan("cs1")]
        pA = C.ps_ring(3, [128, 512], F32, "pA")
        pS = C.ps_ring(2, [128, 512], F32, "pS")
        pO = C.ps([128, 512], F32, "pO")
        pL = C.ps([128, 512], F32, "pL")
        B_pO, B_pL = Buf("pO"), Buf("pL")
        pH = C.ps([128, 512], F32, "pH")
        B_pH = Buf("pH")
        pX = C.ps([128, 512], F32, "pX") if False else None
        f32r = C.sb_ring(12, [128, 512], F32, "f")
        bfr = C.sb_ring(8, [128, 512], BF16, "b")
        ptr = C.sb_ring(3, [128, 512], BF16, "pt")
        qr = C.sb_ring(2, [128, 2, 512], BF16, "q")
        xpr = C.sb_ring(3, [128, 64], BF16, "xp")
        outr = C.sb_ring(2, [128, 3, 512], BF16, "out")
        ch_out = [S.chan("out0"), S.chan("out1")]
        prev_xp = None

        def rms_rstd(src_list, npart, nfeat):
            sq, B_sq = bfr.next()
            pss, B_pss = pA.next()
            for i, (ap, B) in enumerate(src_list):
                S.op("act", [B], [B_sq], lambda e: e.activation(out=sq[0:npart, :], in_=ap, func=AF.Square))
                S.op("pe", [B_sq, Bc], [B_pss],
                     lambda e: e.matmul(pss[0:npart, :], lhsT=ones_bf[0:npart, 0:npart], rhs=sq[0:npart, :],
                                        start=(i == 0), stop=(i == len(src_list) - 1)))
            rstd, B_r = f32r.next()
            S.op("act", [B_pss, Bc], [B_r],
                 lambda e: e.activation(out=rstd[0:npart, :], in_=pss[0:npart, :], func=AF.Ln, scale=1.0 / nfeat,
                                        bias=eps[0:npart, 0:1]))
            S.op("act", [B_r], [B_r],
                 lambda e: e.activation(out=rstd[0:npart, :], in_=rstd[0:npart, :], func=AF.Exp, scale=-0.5))
            return rstd, B_r

        def proj(hb, B_hb, c0, m):
            p, B_p = pA.next()
            for k in range(8):
                S.op("pe", [B_hb, Bc], [B_p],
                     lambda e: e.matmul(p[0:m, :], lhsT=win[:, k, c0:c0 + m], rhs=hb[:, k, :],
                                        start=(k == 0), stop=(k == 7)), inc=(k == 7))
            return p, B_p

        def prep_gen(t):
            nonlocal prev_xp
            bsl = slice(t * 512, (t + 1) * 512)
            hb, B_hb = hring.next()
            S.dma("sp", ch_h[t % 2], [(hb[:], hT[:, bsl].rearrange("(k p) t -> p k t", p=128))], [], [B_hb])
            cs, B_cs = csring.next()
            S.dma("sp", ch_cs[t % 2], [(cs[:], cs2[:, :, bsl])], [], [B_cs])
            ot, B_ot = outr.next()
            q, B_q = qr.next()

            pq0, B_pq0 = proj(hb, B_hb, 512, 128)
            yield
            pq1, B_pq1 = proj(hb, B_hb, 640, 128)
            rstd, B_r = rms_rstd([(pq0[:], B_pq0), (pq1[:], B_pq1)], 128, 256)
            yield
            cqn, B_cqn = bfr.next()
            cqn2, B_cqn2 = bfr.next()
            S.op("dve", [B_pq0, B_r, Bc], [B_cqn],
                 lambda e: e.scalar_tensor_tensor(out=cqn[:], in0=pq0[:], scalar=vec[:, 5:6], in1=rstd[:],
                                                  op0=ALU.mult, op1=ALU.mult))
            S.op("dve", [B_pq1, B_r, Bc], [B_cqn2],
                 lambda e: e.scalar_tensor_tensor(out=cqn2[:], in0=pq1[:], scalar=vec[:, 6:7], in1=rstd[:],
                                                  op0=ALU.mult, op1=ALU.mult))
            cq = [(cqn, B_cqn), (cqn2, B_cqn2)]
            yield
            pqn, B_pqn = pA.next()
            for c in range(2):
                S.op("pe", [cq[c][1], Bc], [B_pqn],
                     lambda e: e.matmul(pqn[:], lhsT=wuq_sb[:, c, 0:128], rhs=cq[c][0][:], start=(c == 0), stop=(c == 1)),
                     inc=(c == 1))
            S.op("act", [B_pqn], [B_q], lambda e: e.activation(out=q[:, 0, :], in_=pqn[:], func=AF.Copy, scale=SCALE))
            yield
            pqp, B_pqp = pA.next()
            for c in range(2):
                S.op("pe", [cq[c][1], Bc], [B_pqp],
                     lambda e: e.matmul(pqp[0:64, :], lhsT=wuq_sb[:, c, 128:192], rhs=cq[c][0][:], start=(c == 0),
                                        stop=(c == 1)), inc=(c == 1))
            pqr, B_pqr = pA.next()
            for c in range(2):
                S.op("pe", [cq[c][1], Bc], [B_pqr],
                     lambda e: e.matmul(pqr[0:64, :], lhsT=wuq_sb[:, c, 192:256], rhs=cq[c][0][:], start=(c == 0),
                                        stop=(c == 1)), inc=(c == 1))
            yield
            t1, B_t1 = f32r.next()
            t2, B_t2 = f32r.next()
            S.op("dve", [B_pqp, B_cs], [B_t1],
                 lambda e: e.scalar_tensor_tensor(out=t1[0:64, :], in0=pqp[0:64, :], scalar=SCALE, in1=cs[:, 0, :],
                                                  op0=ALU.mult, op1=ALU.mult))
            S.op("dve", [B_pqr, B_cs], [B_t2],
                 lambda e: e.scalar_tensor_tensor(out=t2[0:64, :], in0=pqr[0:64, :], scalar=SCALE, in1=cs[:, 1, :],
                                                  op0=ALU.mult, op1=ALU.mult))
            S.op("dve", [B_t1, B_t2], [B_q],
                 lambda e: e.tensor_tensor(out=q[0:64, 1, :], in0=t1[0:64, :], in1=t2[0:64, :], op=ALU.add))
            yield
            pkp, B_pkp = proj(hb, B_hb, 320, 64)
            pkr, B_pkr = proj(hb, B_hb, 384, 64)
            t1, B_t1 = f32r.next()
            t2, B_t2 = f32r.next()
            S.op("dve", [B_pkp, B_cs], [B_t1],
                 lambda e: e.tensor_tensor(out=t1[0:64, :], in0=pkp[0:64, :], in1=cs[:, 0, :], op=ALU.mult))
            S.op("dve", [B_pkr, B_cs], [B_t2],
                 lambda e: e.tensor_tensor(out=t2[0:64, :], in0=pkr[0:64, :], in1=cs[:, 1, :], op=ALU.mult))
            S.op("dve", [B_t1, B_t2], [B_K[t]],
                 lambda e: e.tensor_tensor(out=KpT[:, bsl], in0=t1[0:64, :], in1=t2[0:64, :], op=ALU.add))
            yield
            pkv, B_pkv = proj(hb, B_hb, 768, 128)
            rstd, B_r = rms_rstd([(pkv[:], B_pkv)], 128, 128)
            yield
            ckvn, B_ckvn = bfr.next()
            S.op("dve", [B_pkv, B_r, Bc], [B_ckvn],
                 lambda e: e.scalar_tensor_tensor(out=ckvn[:], in0=pkv[:], scalar=vec[:, 7:8], in1=rstd[:],
                                                  op0=ALU.mult, op1=ALU.mult))
            yield
            pkn, B_pkn = pA.next()
            S.op("pe", [B_ckvn, Bc], [B_pkn],
                 lambda e: e.matmul(pkn[:], lhsT=wukv_sb[:, 0:128], rhs=ckvn[:], start=True, stop=True))
            S.op("act", [B_pkn], [B_K[t]], lambda e: e.activation(out=KnT[:, bsl], in_=pkn[:], func=AF.Copy))
            yield
            pv, B_pv = pA.next()
            for s in range(4):
                S.op("pe", [B_ckvn, Bc], [B_pv],
                     lambda e: e.matmul(pv[:, s * 128:(s + 1) * 128], lhsT=ckvn[:, s * 128:(s + 1) * 128],
                                        rhs=wukv_sb[:, 128:256], start=True, stop=True), inc=(s == 3))
            S.op("act", [B_pv], [B_K[t]],
                 lambda e: e.activation(out=Vt[:, 4 * t:4 * t + 4, :],
                                        in_=pv[:].rearrange("p (s d) -> p s d", s=4), func=AF.Copy))

            yield
            phq, B_phq = proj(hb, B_hb, 0, 128)
            phf, B_phf = proj(hb, B_hb, 128, 128)
            yield
            phg, B_phg = proj(hb, B_hb, 256, 64)
            sig, B_sig = f32r.next()
            S.op("act", [B_phf], [B_sig], lambda e: e.activation(out=sig[:], in_=phf[:], func=AF.Sigmoid))
            qf, B_qf = f32r.next()
            S.op("act", [B_phq], [B_qf], lambda e: e.activation(out=qf[:], in_=phq[:], func=AF.Sigmoid))
            sg, B_sg = f32r.next()
            S.op("act", [B_phg], [B_sg], lambda e: e.activation(out=sg[0:64, :], in_=phg[0:64, :], func=AF.Sigmoid))
            pass
            yield
            S.op("dve", [B_phq, B_qf], [B_qf],
                 lambda e: e.tensor_tensor(out=qf[:], in0=phq[:], in1=qf[:], op=ALU.mult))
            S.op("dve", [B_phg, B_sg], [B_sg],
                 lambda e: e.tensor_tensor(out=sg[0:64, :], in0=phg[0:64, :], in1=sg[0:64, :], op=ALU.mult))
            lf, B_lf = f32r.next()
            S.op("dve", [B_sig, Bc], [B_lf],
                 lambda e: e.tensor_scalar(out=lf[:], in0=sig[:], scalar1=lbv[:, 1:2], scalar2=lbv[:, 0:1],
                                           op0=ALU.mult, op1=ALU.add))
            S.op("dve", [B_lf], [B_lf], lambda e: e.tensor_scalar_max(out=lf[:], in0=lf[:], scalar1=1e-20))
            S.op("act", [B_lf], [B_lf], lambda e: e.activation(out=lf[:], in_=lf[:], func=AF.Ln))
            yield
            kk, B_kk = f32r.next()
            S.op("dve", [B_sig, Bc], [B_kk],
                 lambda e: e.tensor_scalar(out=kk[:], in0=sig[:], scalar1=lbv[:, 2:3], scalar2=lbv[:, 1:2],
                                           op0=ALU.mult, op1=ALU.add))
            bb, B_bb = f32r.next()
            S.op("dve", [B_lf, Bc], [B_bb],
                 lambda e: e.tensor_tensor_scan(out=bb[:], data0=cmask[:], data1=lf[:], initial=0.0,
                                                op0=ALU.mult, op1=ALU.add))
            b3 = bb[:].rearrange("p (c j) -> p c j", j=64)
            yield
            d1, B_d1 = f32r.next()
            d2, B_d2 = f32r.next()
            S.op("dve", [B_bb], [B_d1],
                 lambda e: e.tensor_tensor(out=d1[:].rearrange("p (c j) -> p c j", j=64), in0=b3,
                                           in1=b3[:, :, 31:32].to_broadcast([128, 8, 64]), op=ALU.subtract))
            S.op("dve", [B_bb], [B_d2],
                 lambda e: e.tensor_tensor(out=d2[:].rearrange("p (c j) -> p c j", j=64), in0=b3,
                                           in1=b3[:, :, 63:64].to_broadcast([128, 8, 64]), op=ALU.subtract))
            yield
            e1, B_e1 = f32r.next()
            S.op("act", [B_d1], [B_e1], lambda e: e.activation(out=e1[:], in_=d1[:], func=AF.Exp))
            qt, B_qt = bfr.next()
            S.op("dve", [B_qf, B_e1], [B_qt], lambda e: e.tensor_tensor(out=qt[:], in0=qf[:], in1=e1[:], op=ALU.mult))
            S.op("act", [B_d1], [B_e1], lambda e: e.activation(out=e1[:], in_=d1[:], func=AF.Exp, scale=-1.0))
            yield
            kt, B_kt = bfr.next()
            S.op("dve", [B_kk, B_e1], [B_kt], lambda e: e.tensor_tensor(out=kt[:], in0=kk[:], in1=e1[:], op=ALU.mult))
            S.op("act", [B_d2], [B_d2], lambda e: e.activation(out=d2[:], in_=d2[:], func=AF.Exp, scale=-1.0))
            yield
            kh, B_kh = bfr.next()
            S.op("dve", [B_kk, B_d2], [B_kh], lambda e: e.tensor_tensor(out=kh[:], in0=kk[:], in1=d2[:], op=ALU.mult))
            S.op("act", [B_bb], [B_bb], lambda e: e.activation(out=bb[:], in_=bb[:], func=AF.Exp))
            yield
            qh, B_qh = bfr.next()
            S.op("dve", [B_qf, B_bb], [B_qh], lambda e: e.tensor_tensor(out=qh[:], in0=qf[:], in1=bb[:], op=ALU.mult))
            yield
            pV, B_pV = pA.next()
            for c in range(8):
                csl = slice(c * 64, (c + 1) * 64)
                for k in range(8):
                    S.op("pe", [B_hb, Bc], [B_pV],
                         lambda e: e.matmul(pV[0:64, csl], lhsT=hb[:, k, csl], rhs=win[:, k, 896:960],
                                            start=(k == 0), stop=(k == 7)), inc=(k == 7 and c == 7))
            vt, B_vt = bfr.next()
            S.op("act", [B_pV], [B_vt], lambda e: e.activation(out=vt[0:64, :], in_=pV[0:64, :], func=AF.Copy))
            yield
            pAT, B_pAT = pA.next()
            for c in range(8):
                csl = slice(c * 64, (c + 1) * 64)
                S.op("pe", [B_kt, B_qt], [B_pAT],
                     lambda e: e.matmul(pAT[0:64, csl], lhsT=kt[:, csl], rhs=qt[:, csl], start=True, stop=True),
                     inc=(c == 7))
            at, B_at = bfr.next()
            S.op("dve", [B_pAT, Bc], [B_at],
                 lambda e: e.tensor_tensor(out=at[0:64, :], in0=pAT[0:64, :], in1=tri8[0:64, :], op=ALU.mult))
            yield
            pKT, B_pKT = pA.next()
            pKTb = pKT[:].bitcast(BF16)
            for c in range(8):
                csl = slice(c * 64, (c + 1) * 64)
                S.op("pe", [B_kh, Bc], [B_pKT],
                     lambda e: e.transpose(out=pKTb[0:64, c * 128:(c + 1) * 128], in_=kh[:, csl], identity=ident_bf[:]),
                     inc=(c == 7))
            yield
            kht, B_kht = khtr.next()
            S.op("act", [B_pKT], [B_kht], lambda e: e.activation(out=kht[0:64, :], in_=pKTb[0:64, :], func=AF.Copy))
            for c in range(8):
                csl = slice(c * 64, (c + 1) * 64)
                S.op("pe", [B_kht, B_vt], [B_pH],
                     lambda e: e.matmul(pH[:, csl], lhsT=kht[0:64, c * 128:(c + 1) * 128], rhs=vt[0:64, csl],
                                        start=True, stop=True), inc=(c == 7))
            yield
            pOh, B_pOh = pA.next()
            for c in range(8):
                csl = slice(c * 64, (c + 1) * 64)
                S.op("pe", [B_vt, B_at], [B_pOh],
                     lambda e: e.matmul(pOh[0:64, csl], lhsT=vt[0:64, csl], rhs=at[0:64, csl], start=True, stop=False),
                     inc=False)
                S.op("pe", [B_Sb[c % 2], B_qh], [B_pOh],
                     lambda e: e.matmul(pOh[0:64, csl], lhsT=Sbf[:, c % 2, :], rhs=qh[:, csl], start=False, stop=True))
                S.op("dve", [B_pH, B_bb, B_S], [B_S],
                     lambda e: e.scalar_tensor_tensor(out=Sst[:], in0=Sst[:], scalar=bb[:, c * 64 + 63:c * 64 + 64],
                                                      in1=pH[:, csl], op0=ALU.mult, op1=ALU.add))
                S.op("dve", [B_S], [B_Sb[(c + 1) % 2]],
                     lambda e: e.tensor_copy(out=Sbf[:, (c + 1) % 2, :], in_=Sst[:]))
                yield
            oh, B_oh = f32r.next()
            S.op("act", [B_pOh], [B_oh], lambda e: e.activation(out=oh[0:64, :], in_=pOh[0:64, :], func=AF.Copy))
            yield
            rstd, B_r = rms_rstd([(oh[0:64, :], B_oh)], 64, 64)
            S.op("dve", [B_oh, B_r, Bc], [B_oh],
                 lambda e: e.scalar_tensor_tensor(out=oh[0:64, :], in0=oh[0:64, :], scalar=vec[0:64, 4:5],
                                                  in1=rstd[0:64, :], op0=ALU.mult, op1=ALU.mult))
            S.op("dve", [B_oh, B_sg], [B_ot],
                 lambda e: e.tensor_tensor(out=ot[0:64, 0, :], in0=oh[0:64, :], in1=sg[0:64, :], op=ALU.mult))

            yield
            ppool, B_pp = pA.next()
            for s in range(4):
                gt = 4 * t + s
                for k in range(8):
                    S.op("pe", [B_hb, Bc], [B_pH],
                         lambda e: e.matmul(pH[:, 384:448], lhsT=hb[:, k, s * 128:(s + 1) * 128], rhs=win[:, k, 960:1024],
                                            start=(k == 0), stop=(k == 7)), inc=(k == 7))
                xp, B_xp = xpr.next()
                S.op("act", [B_pH], [B_xp], lambda e: e.activation(out=xp[:], in_=pH[:, 384:448], func=AF.Copy))
                bi = 0 if gt == 0 else 1
                S.op("pe", [B_xp, Bc], [B_pp],
                     lambda e: e.matmul(ppool[0:64, s * 128:(s + 1) * 128], lhsT=xp[:], rhs=band_sb[:, bi, :],
                                        start=True, stop=(gt == 0)), inc=(gt == 0))
                if gt > 0:
                    pxp, B_pxp = prev_xp
                    S.op("pe", [B_pxp, Bc], [B_pp],
                         lambda e: e.matmul(ppool[0:64, s * 128:(s + 1) * 128], lhsT=pxp[:], rhs=band_sb[:, 2, :],
                                            start=False, stop=True))
                prev_xp = (xp, B_xp)
                yield
            yield
            pl_bf, B_plbf = bfr.next()
            S.op("act", [B_pp], [B_plbf], lambda e: e.activation(out=pl_bf[0:64, :], in_=ppool[0:64, :], func=AF.Copy))
            py, B_py = pA.next()
            S.op("pe", [B_plbf, Bc], [B_py],
                 lambda e: e.matmul(py[0:64, :], lhsT=poolw_sb[:, :], rhs=pl_bf[0:64, :], start=True, stop=True))
            S.op("act", [B_py, Bc], [B_ot],
                 lambda e: e.activation(out=ot[0:64, 2, :], in_=py[0:64, :], func=AF.Copy, scale=vec[0:64, 8:9]))

            yield (ot, B_ot, q, B_q)

        def attn(t, res, filler):
            ot, B_ot, q, B_q = res
            bsl = slice(t * 512, (t + 1) * 512)
            nkb = 4 * t + 4

            def qk(kb):
                j = kb - 4 * t
                q0 = 128 * j if j > 0 else 0
                nq = 512 - q0
                ksl = slice(kb * 128, (kb + 1) * 128)
                ps_, B_ps = pS.next()
                S.op("pe", [B_K[kb // 4], B_q], [B_ps],
                     lambda e: e.matmul(ps_[:, 0:nq], lhsT=KnT[:, ksl], rhs=q[:, 0, q0:512], start=True, stop=False),
                     inc=False)
                S.op("pe", [B_K[kb // 4], B_q], [B_ps],
                     lambda e: e.matmul(ps_[:, 0:nq], lhsT=KpT[:, ksl], rhs=q[0:64, 1, q0:512], start=False, stop=True))
                return ps_, B_ps, j, q0, nq

            nxt = qk(0)
            for kb in range(nkb):
                ps_, B_ps, j, q0, nq = nxt
                if kb + 1 < nkb:
                    nxt = qk(kb + 1)
                pt, B_pt = ptr.next()
                S.op("act", [B_ps], [B_pt], lambda e: e.activation(out=pt[:, 0:nq], in_=ps_[:, 0:nq], func=AF.Exp))
                if j >= 0:
                    S.op("dve", [B_pt, Bc], [B_pt],
                         lambda e: e.tensor_tensor(out=pt[:, 0:128], in0=pt[:, 0:128], in1=tri_bf[:], op=ALU.mult))
                S.op("pe", [B_K[kb // 4], B_pt], [B_pO],
                     lambda e: e.matmul(pO[:, q0:512], lhsT=Vt[:, kb, :], rhs=pt[:, 0:nq], start=(kb == 0),
                                        stop=(kb == nkb - 1)), inc=False)
                S.op("pe", [Bc, B_pt], [B_pO, B_pL],
                     lambda e: e.matmul(pL[:, q0:512], lhsT=ones_bf[:], rhs=pt[:, 0:nq], start=(kb == 0),
                                        stop=(kb == nkb - 1)))
                if filler is not None:
                    filler(kb, nkb)
            rl, B_rl = f32r.next()
            S.op("dve", [B_pL], [B_rl], lambda e: e.reciprocal(out=rl[:], in_=pL[:]))
            S.op("dve", [B_pO, B_rl], [B_ot],
                 lambda e: e.tensor_tensor(out=ot[:, 1, :], in0=pO[:], in1=rl[:], op=ALU.mult))
            S.dma("sp", ch_out[t % 2], [(oT[0:64, bsl], ot[0:64, 0, :]), (oT[64:192, bsl], ot[:, 1, :]),
                                        (oT[192:256, bsl], ot[0:64, 2, :])], [B_ot], [])

        def run_gen(g):
            r = None
            while True:
                try:
                    v = next(g)
                    if v is not None:
                        r = v
                except StopIteration:
                    return r

        NY = 48
        res = run_gen(prep_gen(0))
        for t in range(NBLK):
            nres = [None]
            if t + 1 < NBLK:
                g = prep_gen(t + 1)
                done = [False]

                def filler(kb, nkb, g=g, done=done, nres=nres):
                    if done[0]:
                        return
                    steps = 1 if nkb >= NY else -(-NY // nkb)
                    for _ in range(steps):
                        try:
                            v = next(g)
                            if v is not None:
                                nres[0] = v
                        except StopIteration:
                            done[0] = True
                            return
                attn(t, res, filler)
                if not done[0]:
                    r = run_gen(g)
                    if r is not None:
                        nres[0] = r
            else:
                attn(t, res, None)
            res = nres[0]
        S.finish("sp")
    return nc


POOL_WINDOWS = (2, 4, 8, 16)


def _gl(g):
    return np.ascontiguousarray(np.asarray(g, np.float32).reshape(8, 128).T)


def _rope_tables(ns):
    pos = np.arange(ns, dtype=np.float32)
    inv_freq = (1.0 / (np.float32(10000.0) ** (np.arange(0, 64, 2, dtype=np.float32) / np.float32(64)))).astype(np.float32)
    ang = (pos[:, None] * inv_freq[None, :]).astype(np.float32)
    cos = np.cos(ang).astype(np.float32).T
    sin = np.sin(ang).astype(np.float32).T
    cs2 = np.empty((64, 2, ns), np.float32)
    cs2[0:32, 0] = cos
    cs2[32:64, 0] = cos
    cs2[0:32, 1] = -sin
    cs2[32:64, 1] = sin
    return cs2


def _band(w):
    b = np.zeros((128, 3, 128), np.float32)
    s = np.arange(128)[:, None]
    t = np.arange(128)[None, :]
    win = (s <= t) & (s > t - w)
    eye = (s == t).astype(np.float32)
    cnt = np.minimum(t + 1, w).astype(np.float32)
    b[:, 0, :] = win / cnt - eye
    b[:, 1, :] = win / np.float32(w) - eye
    b[:, 2, :] = ((s - 128) > (t - w)).astype(np.float32) / np.float32(w)
    return b


def _consts():
    c = np.zeros((128, 3, 512), np.float32)
    c[:, 0, 0:128] = np.eye(128, dtype=np.float32)
    k = np.arange(128)[:, None]
    q = np.arange(128)[None, :]
    c[:, 1, 0:128] = (q >= k).astype(np.float32)
    c[:, 2, :] = 1.0
    c[:, 2, 0::64] = 0.0
    return c


def prep_B(inp, l, r, ns):
    w_in = np.asarray(inp["w_in"][l], np.float32)
    winh = np.zeros((D, 1024), np.float32)
    winh[:, 0:128] = w_in[:, r * 128:(r + 1) * 128]
    winh[:, 128:256] = w_in[:, 512 + r * 128:512 + (r + 1) * 128]
    winh[:, 256:320] = w_in[:, 1280 + r * 64:1280 + (r + 1) * 64]
    kpe = w_in[:, 1920:1984]
    winh[:, 320:384] = kpe
    winh[:, 384:416] = kpe[:, 32:64]
    winh[:, 416:448] = kpe[:, 0:32]
    winh[:, 512:768] = w_in[:, 1536:1792]
    winh[:, 768:896] = w_in[:, 1792:1920]
    winh[:, 896:960] = w_in[:, 1024 + r * 64:1024 + (r + 1) * 64]
    winh[:, 960:1024] = w_in[:, 1984 + r * 64:1984 + (r + 1) * 64]
    uq = np.asarray(inp["mla_w_uq"][l], np.float32)[:, r * 192:(r + 1) * 192]
    wuq = np.zeros((256, 256), np.float32)
    wuq[:, 0:192] = uq
    wuq[:, 192:224] = uq[:, 160:192]
    wuq[:, 224:256] = uq[:, 128:160]
    wukv = np.ascontiguousarray(np.asarray(inp["mla_w_ukv"][l], np.float32)[:, r * 256:(r + 1) * 256])
    poolw = np.ascontiguousarray(np.asarray(inp["pool_w"][l, r], np.float32))
    vecs = np.zeros((128, 16), np.float32)
    lbr = np.asarray(inp["hgrn_lower_bounds"], np.float32)
    vecs[:, 1] = lbr[0, r * 128:(r + 1) * 128]
    vecs[:, 2] = lbr[1, r * 128:(r + 1) * 128]
    vecs[:, 3] = 1.0 if l == 1 else 0.0
    vecs[0:64, 4] = np.asarray(inp["hgrn_out_norm"][l], np.float32)[r * 64:(r + 1) * 64]
    vecs[:, 5] = np.asarray(inp["mla_q_norm"][l], np.float32)[0:128]
    vecs[:, 6] = np.asarray(inp["mla_q_norm"][l], np.float32)[128:256]
    vecs[:, 7] = np.asarray(inp["mla_kv_norm"][l], np.float32)
    vecs[0:64, 8] = np.asarray(inp["pool_scale"][l], np.float32)[r * 64:(r + 1) * 64]
    return dict(winh=winh, wuq=wuq, wukv=wukv, poolw=poolw, vecs=vecs, cs2=_rope_tables(ns),
                band=_band(POOL_WINDOWS[r]), consts=_consts())


_PROGS = {}


def _prog(key, fn):
    if key not in _PROGS:
        _PROGS[key] = fn()
    return _PROGS[key]


def _wo_perm(w_o):
    idx = []
    for r in range(4):
        idx += list(range(r * 64, (r + 1) * 64))
        idx += list(range(256 + r * 128, 256 + (r + 1) * 128))
        idx += list(range(768 + r * 64, 768 + (r + 1) * 64))
    return np.ascontiguousarray(np.asarray(w_o, np.float32)[np.asarray(idx)])


def _run(nc, in_maps):
    res = run_bass_kernel_spmd(nc, in_maps, core_ids=list(range(NCORES)))
    return res.results


def kernel(**inp):
    inp = {k: np.asarray(v) for k, v in inp.items()}
    x = np.asarray(inp["x"], np.float32).reshape(BATCH * SEQ, D)
    ident = np.eye(128, dtype=np.float32)
    ncA = _prog("A", build_A)
    gv = np.concatenate([_gl(inp["attn_norm"][0]), _gl(inp["attn_norm"][0])], axis=1)
    xTs = [np.ascontiguousarray(x[c * TOK:(c + 1) * TOK].T) for c in range(NCORES)]
    rA = _run(ncA, [dict(xT=xTs[c], gvec=gv) for c in range(NCORES)])
    hTs = [rA[c]["hT"] for c in range(NCORES)]
    out = None
    for l in range(2):
        ncB = _prog("B", build_B)
        hfull = [np.ascontiguousarray(np.concatenate(hTs[4 * b:4 * b + 4], axis=1)) for b in range(BATCH)]
        insB = []
        for c in range(NCORES):
            d = prep_B(inp, l, c % 4, SEQ)
            d["hT"] = hfull[c // 4]
            insB.append(d)
        rB = _run(ncB, insB)
        wo = _wo_perm(inp["w_o"][l])
        insC = []
        for c in range(NCORES):
            b, qd = c // 4, c % 4
            oT = np.ascontiguousarray(np.concatenate(
                [rB[4 * b + r]["oTo"][:, qd * TOK:(qd + 1) * TOK] for r in range(4)], axis=0))
            d = dict(xT=xTs[c], oT=oT, wo=wo, ident=ident)
            if l == 0:
                d["gvec"] = np.concatenate([_gl(inp["ffn_norm"][0]), _gl(inp["attn_norm"][1])], axis=1)
                d["wg"] = np.asarray(inp["dense_w_gate"][0], np.float32)
                d["wu"] = np.asarray(inp["dense_w_up"][0], np.float32)
                d["wd"] = np.asarray(inp["dense_w_down"][0], np.float32)
            else:
                d["gvec"] = np.concatenate([_gl(inp["ffn_norm"][1]), _gl(inp["final_norm"])], axis=1)
                d["wg"] = np.asarray(inp["moe_w_gate"][0], np.float32)
                d["wu"] = np.asarray(inp["moe_w_up"][0], np.float32)
                d["wd"] = np.asarray(inp["moe_w_down"][0], np.float32)
                d["router"] = np.asarray(inp["moe_router"][0], np.float32)
            insC.append(d)
        if l == 0:
            rC = _run(_prog("C0", lambda: build_C("dense", False)), insC)
            xTs = [rC[c]["x2T"] for c in range(NCORES)]
            hTs = [rC[c]["hnT"] for c in range(NCORES)]
        else:
            rC = _run(_prog("C1", lambda: build_C("moe", True)), insC)
            out = np.concatenate([rC[c]["outT"].T for c in range(NCORES)], axis=0)
    return np.ascontiguousarray(out.reshape(BATCH, SEQ, D).astype(np.float32))
```

```python
import numpy as np
from contextlib import ExitStack
import ml_dtypes
import concourse.bass as bass
import concourse.mybir as mybir
from concourse.bass_utils import run_bass_kernel_spmd

F32 = mybir.dt.float32
BF16 = mybir.dt.bfloat16
AF = mybir.ActivationFunctionType
ALU = mybir.AluOpType
AX = mybir.AxisListType

NCORES = 8
D = 1024
SEQ = 16384
BATCH = 2
TOK = 4096
DFF = 2560
DFFE = 3584
NEXP = 8
EPS = 1e-6
D_IN = 2240


class Buf:
    __slots__ = ("name", "w", "r")

    def __init__(self, name=""):
        self.name = name
        self.w = None
        self.r = []


class Chan:
    __slots__ = ("sem", "total", "id")


class Sync:
    def __init__(self, nc, stack):
        self.nc = nc
        self.stack = stack
        self.eng = {"pe": nc.tensor, "act": nc.scalar, "dve": nc.vector,
                    "pool": nc.gpsimd, "sp": nc.sync}
        self.sem = {}
        self.cnt = {}
        self.seen = {}
        for k in self.eng:
            self.sem[k] = stack.enter_context(nc.semaphore("s_" + k))
            self.cnt[k] = 0
            self.seen[k] = {}
        self.chans = []

    def chan(self, name=""):
        c = Chan()
        c.sem = self.stack.enter_context(self.nc.semaphore("d%d_%s" % (len(self.chans), name)))
        c.total = 0
        c.id = len(self.chans)
        self.chans.append(c)
        return c

    def _wait(self, e, ev):
        kind, key, val = ev
        if kind == "e" and key == e and e == "pe":
            return
        k = (kind, key if kind == "e" else key.id)
        if self.seen[e].get(k, 0) >= val:
            return
        self.seen[e][k] = val
        sem = self.sem[key] if kind == "e" else key.sem
        self.eng[e].wait_ge(sem, val)

    def _deps(self, e, reads, writes):
        for b in reads:
            if b.w is not None:
                self._wait(e, b.w)
        for b in writes:
            if b.w is not None:
                self._wait(e, b.w)
            for ev in b.r:
                if ev[0] == "e" and ev[1] == e:
                    continue
                self._wait(e, ev)

    def _mark(self, ev, reads, writes):
        for b in writes:
            b.w = ev
            b.r = []
        for b in reads:
            if b in writes:
                continue
            b.r = [x for x in b.r if not (x[0] == ev[0] and x[1] is ev[1])]
            b.r.append(ev)

    def op(self, e, reads, writes, fn, inc=True):
        self._deps(e, reads, writes)
        ins = fn(self.eng[e])
        if inc:
            ins.then_inc(self.sem[e], 1)
            self.cnt[e] += 1
            ev = ("e", e, self.cnt[e])
        else:
            ev = ("e", e, self.cnt[e] + 1)
        self._mark(ev, reads, writes)
        return ins

    def dma(self, q, ch, pairs, reads, writes, **kw):
        self._deps(q, reads, writes)
        for (o, i) in pairs:
            ins = self.eng[q].dma_start(out=o, in_=i, **kw)
            ins.then_inc(ch.sem, 16)
            ch.total += 16
        ev = ("d", ch, ch.total)
        self._mark(ev, reads, writes)

    def barrier(self):
        for e in self.eng:
            for f in self.eng:
                if f != e and self.cnt[f] > 0:
                    self._wait(e, ("e", f, self.cnt[f]))
            for c in self.chans:
                if c.total > 0:
                    self._wait(e, ("d", c, c.total))

    def finish(self, e="sp"):
        for c in self.chans:
            if c.total > 0:
                self._wait(e, ("d", c, c.total))
        for f in self.eng:
            if f != e and self.cnt[f] > 0:
                self._wait(e, ("e", f, self.cnt[f]))


class Ring:
    def __init__(self, items):
        self.items = items
        self.i = 0

    def next(self):
        it = self.items[self.i % len(self.items)]
        self.i += 1
        return it


class Ctx:
    def __init__(self, nc, stack):
        self.nc = nc
        self.st = stack
        self.S = Sync(nc, stack)
        self.n = 0

    def sb(self, shape, dt, name=None):
        self.n += 1
        t = self.st.enter_context(self.nc.sbuf_tensor("S_" + (name or ("sb%d" % self.n)), list(shape), dt))
        return t

    def ps(self, shape, dt=F32, name=None):
        self.n += 1
        t = self.st.enter_context(self.nc.psum_tensor("P_" + (name or ("ps%d" % self.n)), list(shape), dt))
        return t

    def sb_ring(self, n, shape, dt, name):
        return Ring([(self.sb(shape, dt, "%s%d" % (name, i)), Buf("%s%d" % (name, i))) for i in range(n)])

    def ps_ring(self, n, shape, dt, name):
        return Ring([(self.ps(shape, dt, "%s%d" % (name, i)), Buf("%s%d" % (name, i))) for i in range(n)])


def emit_rmsnorm(C, K, xs, B_xs, g_sb, out_fn, NT):
    S = C.S
    for tt in range(NT // 512):
        sl = slice(tt * 512, (tt + 1) * 512)
        sq, B_sq = K["sq"].next()
        for oc in range(8):
            S.op("act", [B_xs[oc][tt]], [B_sq],
                 lambda e: e.activation(out=sq[:, oc, :], in_=xs[:, oc, sl], func=AF.Square))
        pss, B_pss = K["pmisc"].next()
        for oc in range(8):
            S.op("pe", [B_sq, K["B_const"]], [B_pss],
                 lambda e: e.matmul(pss[:], lhsT=K["ones_bf"][:], rhs=sq[:, oc, :],
                                    start=(oc == 0), stop=(oc == 7)), inc=(oc == 7))
        rstd, B_rstd = K["rstd"].next()
        S.op("act", [B_pss, K["B_const"]], [B_rstd],
             lambda e: e.activation(out=rstd[:], in_=pss[:], func=AF.Ln, scale=1.0 / D,
                                    bias=K["eps"][:, 0:1]))
        S.op("act", [B_rstd], [B_rstd],
             lambda e: e.activation(out=rstd[:], in_=rstd[:], func=AF.Exp, scale=-0.5))
        for oc in range(8):
            o_ap, wb = out_fn(oc, tt)
            S.op("dve", [B_xs[oc][tt], B_rstd, K["B_const"]], wb,
                 lambda e: e.scalar_tensor_tensor(out=o_ap, in0=xs[:, oc, sl], scalar=g_sb[:, oc:oc + 1],
                                                  in1=rstd[:], op0=ALU.mult, op1=ALU.mult))


def emit_down(C, K, src, B_src, nf, wd, xs, B_xs, NT, bc=None, B_bc=None):
    S = C.S
    for oc in range(8):
        dsl, B_d = K["dslot"].next()
        S.dma("pool", K["ch_d"][(K["dslot"].i - 1) % len(K["ch_d"])],
              [(dsl[:, 0:nf, :], wd[:, oc * 128:(oc + 1) * 128].rearrange("(f p) n -> p f n", p=128))],
              [], [B_d])
        for tt in range(NT // 512):
            sl = slice(tt * 512, (tt + 1) * 512)
            py, B_py = K["py"].next()
            for f in range(nf):
                S.op("pe", [B_d, B_src[tt]], [B_py],
                     lambda e: e.matmul(py[:], lhsT=dsl[:, f, :], rhs=src[:, f, sl],
                                        start=(f == 0), stop=(f == nf - 1)), inc=(f == nf - 1))
            if bc is None:
                S.op("dve", [B_py, B_xs[oc][tt]], [B_xs[oc][tt]],
                     lambda e: e.tensor_tensor(out=xs[:, oc, sl], in0=py[:], in1=xs[:, oc, sl], op=ALU.add))
            else:
                tmp, B_tmp = K["sil"].next()
                S.op("dve", [B_py, B_bc], [B_tmp],
                     lambda e: e.tensor_tensor(out=tmp[:], in0=py[:], in1=bc[:, sl], op=ALU.mult))
                S.op("dve", [B_tmp, B_xs[oc][tt]], [B_xs[oc][tt]],
                     lambda e: e.tensor_tensor(out=xs[:, oc, sl], in0=tmp[:], in1=xs[:, oc, sl], op=ALU.add))


def emit_gate_up(C, K, h2, B_h2, wg, wu, nf, a, B_a, NT):
    S = C.S
    for f in range(nf):
        gu, B_gu = K["guslot"].next()
        S.dma("pool", K["ch_gu"][(K["guslot"].i - 1) % len(K["ch_gu"])],
              [(gu[:, :, 0:128], wg[:, f * 128:(f + 1) * 128].rearrange("(k p) n -> p k n", p=128)),
               (gu[:, :, 128:256], wu[:, f * 128:(f + 1) * 128].rearrange("(k p) n -> p k n", p=128))],
              [], [B_gu])
        for tt in range(NT // 512):
            sl = slice(tt * 512, (tt + 1) * 512)
            pg, B_pg = K["pg"].next()
            pu, B_pu = K["pu"].next()
            for k in range(8):
                S.op("pe", [B_gu, B_h2[tt]], [B_pg],
                     lambda e: e.matmul(pg[:], lhsT=gu[:, k, 0:128], rhs=h2[:, k, sl],
                                        start=(k == 0), stop=(k == 7)), inc=(k == 7))
            for k in range(8):
                S.op("pe", [B_gu, B_h2[tt]], [B_pu],
                     lambda e: e.matmul(pu[:], lhsT=gu[:, k, 128:256], rhs=h2[:, k, sl],
                                        start=(k == 0), stop=(k == 7)), inc=(k == 7))
            sil, B_sil = K["sil"].next()
            S.op("act", [B_pg], [B_sil],
                 lambda e: e.activation(out=sil[:], in_=pg[:], func=AF.Silu))
            S.op("dve", [B_sil, B_pu], [B_a[tt]],
                 lambda e: e.tensor_tensor(out=a[:, f, sl], in0=pu[:], in1=sil[:], op=ALU.mult))


def emit_router(C, K, h2, B_h2, rt_sb, combT, B_combT, NT):
    S = C.S
    nc = C.nc
    for s in range(NT // 128):
        tt = s // 4
        tsl = slice(s * 128, (s + 1) * 128)
        pl, B_pl = K["pmisc"].next()
        for k in range(8):
            S.op("pe", [B_h2[tt], K["B_const"]], [B_pl],
                 lambda e: e.matmul(pl[:, 0:8], lhsT=h2[:, k, tsl], rhs=rt_sb[:, k, :],
                                    start=(k == 0), stop=(k == 7)), inc=(k == 7))
        r, B_r = K["rt"].next()
        S.op("act", [B_pl], [B_r], lambda e: e.activation(out=r[:, 0:8], in_=pl[:, 0:8], func=AF.Copy))
        S.op("dve", [B_r], [B_r], lambda e: e.max(out=r[:, 8:16], in_=r[:, 0:8]))
        S.op("dve", [B_r], [B_r], lambda e: e.tensor_scalar(out=r[:, 16:24], in0=r[:, 0:8], scalar1=r[:, 9:10],
                                                            scalar2=None, op0=ALU.is_ge))
        S.op("dve", [B_r], [B_r], lambda e: e.tensor_scalar(out=r[:, 32:33], in0=r[:, 8:9], scalar1=-1.0,
                                                            scalar2=None, op0=ALU.mult))
        S.op("act", [B_r], [B_r], lambda e: e.activation(out=r[:, 24:32], in_=r[:, 0:8], func=AF.Exp,
                                                         bias=r[:, 32:33], scale=1.0))
        S.op("dve", [B_r], [B_r], lambda e: e.tensor_tensor(out=r[:, 24:32], in0=r[:, 24:32], in1=r[:, 16:24],
                                                            op=ALU.mult))
        S.op("dve", [B_r], [B_r], lambda e: e.reduce_sum(out=r[:, 33:34], in_=r[:, 24:32], axis=AX.X))
        S.op("dve", [B_r], [B_r], lambda e: e.reciprocal(out=r[:, 34:35], in_=r[:, 33:34]))
        S.op("dve", [B_r], [B_r], lambda e: e.tensor_scalar(out=r[:, 40:48], in0=r[:, 24:32], scalar1=r[:, 34:35],
                                                            scalar2=None, op0=ALU.mult))
        pt, B_pt = K["pmisc"].next()
        S.op("pe", [B_r, K["B_const"]], [B_pt],
             lambda e: e.transpose(out=pt[0:8, 0:128], in_=r[:, 40:48], identity=K["ident"][:]))
        S.op("act", [B_pt], [B_combT],
             lambda e: e.activation(out=combT[0:8, tsl], in_=pt[0:8, 0:128], func=AF.Copy))


def build_C(kind, final, NT=1024):
    nc = bass.Bass("TRN2", target_bir_lowering=False)
    dt = nc.dram_tensor
    xT = dt("xT", [D, TOK], F32, kind="ExternalInput").ap()
    oT = dt("oT", [D, TOK], BF16, kind="ExternalInput").ap()
    wo = dt("wo", [D, D], F32, kind="ExternalInput").ap()
    gvec = dt("gvec", [128, 16], F32, kind="ExternalInput").ap()
    ident_d = dt("ident", [128, 128], F32, kind="ExternalInput").ap()
    if kind == "dense":
        wg = dt("wg", [D, DFF], F32, kind="ExternalInput").ap()
        wu = dt("wu", [D, DFF], F32, kind="ExternalInput").ap()
        wd = dt("wd", [DFF, D], F32, kind="ExternalInput").ap()
        nfmax = DFF // 128
    else:
        wg = dt("wg", [NEXP, D, DFFE], F32, kind="ExternalInput").ap()
        wu = dt("wu", [NEXP, D, DFFE], F32, kind="ExternalInput").ap()
        wd = dt("wd", [NEXP, DFFE, D], F32, kind="ExternalInput").ap()
        rt = dt("router", [D, NEXP], F32, kind="ExternalInput").ap()
        nfmax = DFFE // 128
    if final:
        outT = dt("outT", [D, TOK], F32, kind="ExternalOutput").ap()
    else:
        x2T = dt("x2T", [D, TOK], F32, kind="ExternalOutput").ap()
        hnT = dt("hnT", [D, TOK], BF16, kind="ExternalOutput").ap()

    with ExitStack() as st:
        C = Ctx(nc, st)
        S = C.S
        TT = NT // 512
        K = {}
        K["B_const"] = Buf("const")
        K["ones_bf"] = C.sb([128, 128], BF16, "ones_bf")
        K["eps"] = C.sb([128, 1], F32, "eps")
        K["ident"] = C.sb([128, 128], F32, "ident")
        g_sb = C.sb([128, 16], F32, "g_sb")
        ch_c = S.chan("const")
        ch_cp = S.chan("constp")
        S.op("dve", [], [K["B_const"]], lambda e: e.memset(K["ones_bf"][:], 1.0))
        S.op("dve", [], [K["B_const"]], lambda e: e.memset(K["eps"][:], EPS))
        S.dma("sp", ch_c, [(g_sb[:], gvec), (K["ident"][:], ident_d)], [], [K["B_const"]])
        if kind == "moe":
            rt_sb = C.sb([128, 8, 8], BF16, "rt_sb")
            S.dma("pool", ch_cp, [(rt_sb[:], rt.rearrange("(k p) e -> p k e", p=128))], [], [K["B_const"]])
            esel = C.sb([8, 8, 128], F32, "esel")
            for e_ in range(8):
                S.op("dve", [K["B_const"]], [K["B_const"]],
                     lambda e: e.tensor_copy(out=esel[0:8, e_, :],
                                             in_=K["ident"][0:8, e_:e_ + 1].to_broadcast([8, 128])))
            combT = C.sb([8, NT], F32, "combT")
            B_combT = Buf("combT")
            bc = C.sb([128, NT], F32, "bc")
            B_bc = Buf("bc")
            K["rt"] = C.sb_ring(2, [128, 48], F32, "rts")
        xs = C.sb([128, 8, NT], F32, "xs")
        B_xs = [[Buf("xs%d_%d" % (oc, tt)) for tt in range(TT)] for oc in range(8)]
        osb = C.sb([128, 8, NT], BF16, "osb")
        B_os = [Buf("os%d" % tt) for tt in range(TT)]
        h2 = C.sb([128, 8, NT], BF16, "h2")
        B_h2 = [Buf("h2_%d" % tt) for tt in range(TT)]
        a = C.sb([128, nfmax, NT], BF16, "a")
        B_a = [Buf("a%d" % tt) for tt in range(TT)]
        K["sq"] = C.sb_ring(2, [128, 8, 512], BF16, "sq")
        K["rstd"] = C.sb_ring(2, [128, 512], F32, "rstd")
        K["sil"] = C.sb_ring(3, [128, 512], F32, "sil")
        K["guslot"] = C.sb_ring(5, [128, 8, 256], BF16, "gu")
        K["ch_gu"] = [S.chan("gu%d" % i) for i in range(5)]
        K["dslot"] = C.sb_ring(3, [128, nfmax, 128], BF16, "dsl")
        K["ch_d"] = [S.chan("d%d" % i) for i in range(3)]
        K["pg"] = C.ps_ring(2, [128, 512], F32, "pg")
        K["pu"] = C.ps_ring(2, [128, 512], F32, "pu")
        K["py"] = C.ps_ring(2, [128, 512], F32, "py")
        K["pmisc"] = C.ps_ring(2, [128, 512], F32, "pm")
        ch_x = S.chan("x")
        ch_o = S.chan("o")
        ch_out = S.chan("out")
        if final:
            ostage = C.sb_ring(2, [128, 512], F32, "ost")
        else:
            hn = C.sb([128, 8, NT], BF16, "hn")
            B_hn = [Buf("hn%d" % tt) for tt in range(TT)]

        for sti in range(TOK // NT):
            t0 = sti * NT
            S.dma("sp", ch_x, [(xs[:, :, tt * 512:(tt + 1) * 512],
                                xT[:, t0 + tt * 512:t0 + (tt + 1) * 512].rearrange("(k p) t -> p k t", p=128))
                               for tt in range(TT)],
                  [], [B_xs[oc][tt] for oc in range(8) for tt in range(TT)])
            S.dma("sp", ch_o, [(osb[:, :, tt * 512:(tt + 1) * 512],
                                oT[:, t0 + tt * 512:t0 + (tt + 1) * 512].rearrange("(k p) t -> p k t", p=128))
                               for tt in range(TT)],
                  [], B_os)
            emit_down(C, K, osb, B_os, 8, wo, xs, B_xs, NT)
            emit_rmsnorm(C, K, xs, B_xs, g_sb, lambda oc, tt: (h2[:, oc, tt * 512:(tt + 1) * 512], [B_h2[tt]]), NT)
            if kind == "dense":
                emit_gate_up(C, K, h2, B_h2, wg, wu, DFF // 128, a, B_a, NT)
                emit_down(C, K, a, B_a, DFF // 128, wd, xs, B_xs, NT)
            else:
                emit_router(C, K, h2, B_h2, rt_sb, combT, B_combT, NT)
                for ex in range(NEXP):
                    for tt in range(TT):
                        sl = slice(tt * 512, (tt + 1) * 512)
                        pb, B_pb = K["pmisc"].next()
                        S.op("pe", [B_combT, K["B_const"]], [B_pb],
                             lambda e: e.matmul(pb[:], lhsT=esel[0:8, ex, :], rhs=combT[0:8, sl],
                                                start=True, stop=True))
                        S.op("act", [B_pb], [B_bc],
                             lambda e: e.activation(out=bc[:, sl], in_=pb[:], func=AF.Copy))
                    emit_gate_up(C, K, h2, B_h2, wg[ex], wu[ex], DFFE // 128, a, B_a, NT)
                    emit_down(C, K, a, B_a, DFFE // 128, wd[ex], xs, B_xs, NT, bc=bc, B_bc=B_bc)
            if final:
                def out_fn(oc, tt):
                    return None
                stage_list = {}

                def fin_out(oc, tt):
                    t_, b_ = ostage.next()
                    stage_list[(oc, tt)] = (t_, b_)
                    return (t_[:], [b_])
                _emit_final(C, K, xs, B_xs, g_sb, ostage, outT, t0, ch_out, NT)
            else:
                S.dma("sp", ch_out, [(x2T[:, t0 + tt * 512:t0 + (tt + 1) * 512].rearrange("(k p) t -> p k t", p=128),
                                      xs[:, :, tt * 512:(tt + 1) * 512]) for tt in range(TT)],
                      [B_xs[oc][tt] for oc in range(8) for tt in range(TT)], [])
                emit_rmsnorm(C, K, xs, B_xs, g_sb[:, 8:16],
                             lambda oc, tt: (hn[:, oc, tt * 512:(tt + 1) * 512], [B_hn[tt]]), NT)
                S.dma("sp", ch_out, [(hnT[:, t0 + tt * 512:t0 + (tt + 1) * 512].rearrange("(k p) t -> p k t", p=128),
                                      hn[:, :, tt * 512:(tt + 1) * 512]) for tt in range(TT)],
                      B_hn, [])
        S.finish("sp")
    return nc


def _emit_final(C, K, xs, B_xs, g_sb, ostage, outT, t0, ch_out, NT):
    S = C.S
    for tt in range(NT // 512):
        sl = slice(tt * 512, (tt + 1) * 512)
        sq, B_sq = K["sq"].next()
        for oc in range(8):
            S.op("act", [B_xs[oc][tt]], [B_sq],
                 lambda e: e.activation(out=sq[:, oc, :], in_=xs[:, oc, sl], func=AF.Square))
        pss, B_pss = K["pmisc"].next()
        for oc in range(8):
            S.op("pe", [B_sq, K["B_const"]], [B_pss],
                 lambda e: e.matmul(pss[:], lhsT=K["ones_bf"][:], rhs=sq[:, oc, :],
                                    start=(oc == 0), stop=(oc == 7)), inc=(oc == 7))
        rstd, B_rstd = K["rstd"].next()
        S.op("act", [B_pss, K["B_const"]], [B_rstd],
             lambda e: e.activation(out=rstd[:], in_=pss[:], func=AF.Ln, scale=1.0 / D, bias=K["eps"][:, 0:1]))
        S.op("act", [B_rstd], [B_rstd],
             lambda e: e.activation(out=rstd[:], in_=rstd[:], func=AF.Exp, scale=-0.5))
        for oc in range(8):
            ot, B_ot = ostage.next()
            S.op("dve", [B_xs[oc][tt], B_rstd, K["B_const"]], [B_ot],
                 lambda e: e.scalar_tensor_tensor(out=ot[:], in0=xs[:, oc, sl], scalar=g_sb[:, 8 + oc:9 + oc],
                                                  in1=rstd[:], op0=ALU.mult, op1=ALU.mult))
            S.dma("sp", ch_out, [(outT[oc * 128:(oc + 1) * 128, t0 + tt * 512:t0 + (tt + 1) * 512], ot[:])],
                  [B_ot], [])


def build_A():
    nc = bass.Bass("TRN2", target_bir_lowering=False)
    xT = nc.dram_tensor("xT", [D, TOK], F32, kind="ExternalInput").ap()
    gvec = nc.dram_tensor("gvec", [128, 16], F32, kind="ExternalInput").ap()
    hT = nc.dram_tensor("hT", [D, TOK], BF16, kind="ExternalOutput").ap()
    with ExitStack() as st:
        C = Ctx(nc, st)
        S = C.S
        K = {}
        K["B_const"] = Buf("const")
        K["ones_bf"] = C.sb([128, 128], BF16, "ones_bf")
        K["eps"] = C.sb([128, 1], F32, "eps")
        g_sb = C.sb([128, 16], F32, "g_sb")
        ch_c = S.chan("const")
        ch_cp = S.chan("constp")
        S.op("dve", [], [K["B_const"]], lambda e: e.memset(K["ones_bf"][:], 1.0))
        S.op("dve", [], [K["B_const"]], lambda e: e.memset(K["eps"][:], EPS))
        S.dma("sp", ch_c, [(g_sb[:], gvec)], [], [K["B_const"]])
        K["sq"] = C.sb_ring(2, [128, 8, 512], BF16, "sq")
        K["rstd"] = C.sb_ring(2, [128, 512], F32, "rstd")
        K["pmisc"] = C.ps_ring(2, [128, 512], F32, "pm")
        xr = C.sb_ring(2, [128, 8, 512], F32, "xs")
        hr = C.sb_ring(2, [128, 8, 512], BF16, "hs")
        chx = [S.chan("x0"), S.chan("x1")]
        cho = [S.chan("o0"), S.chan("o1")]
        for j in range(TOK // 512):
            xs, B = xr.next()
            hs, Bh = hr.next()
            S.dma("sp", chx[j % 2], [(xs[:], xT[:, j * 512:(j + 1) * 512].rearrange("(k p) t -> p k t", p=128))],
                  [], [B])
            B_xs = [[B] for _ in range(8)]
            emit_rmsnorm(C, K, xs, B_xs, g_sb, lambda oc, tt: (hs[:, oc, :], [Bh]), 512)
            S.dma("sp", cho[j % 2], [(hT[:, j * 512:(j + 1) * 512].rearrange("(k p) t -> p k t", p=128), hs[:])],
                  [Bh], [])
        S.finish("sp")
    return nc


SCALE = (128 + 64) ** -0.5


def build_B(NBLK=32):
    NS = NBLK * 512
    nc = bass.Bass("TRN2", target_bir_lowering=False)
    dt = nc.dram_tensor
    hT = dt("hT", [D, NS], BF16, kind="ExternalInput").ap()
    winh = dt("winh", [D, 1024], F32, kind="ExternalInput").ap()
    wuq = dt("wuq", [256, 256], F32, kind="ExternalInput").ap()
    wukv = dt("wukv", [128, 256], F32, kind="ExternalInput").ap()
    poolw = dt("poolw", [64, 64], F32, kind="ExternalInput").ap()
    vecs = dt("vecs", [128, 16], F32, kind="ExternalInput").ap()
    cs2 = dt("cs2", [64, 2, NS], F32, kind="ExternalInput").ap()
    band = dt("band", [128, 3, 128], F32, kind="ExternalInput").ap()
    consts = dt("consts", [128, 3, 512], F32, kind="ExternalInput").ap()
    oT = dt("oTo", [256, NS], BF16, kind="ExternalOutput").ap()

    with ExitStack() as st:
        C = Ctx(nc, st)
        S = C.S
        Bc = Buf("const")
        ch_c = S.chan("const")
        ch_cp = S.chan("constp")
        pass
        win = C.sb([128, 8, 1024], BF16, "win")
        S.dma("pool", ch_cp, [(win[:, k, :], winh[k * 128:(k + 1) * 128, :]) for k in range(8)], [], [Bc])
        wuq_sb = C.sb([128, 2, 256], BF16, "wuq")
        S.dma("pool", ch_cp, [(wuq_sb[:], wuq.rearrange("(k p) n -> p k n", p=128))], [], [Bc])
        wukv_sb = C.sb([128, 256], BF16, "wukv")
        poolw_sb = C.sb([64, 64], BF16, "poolw")
        band_sb = C.sb([128, 3, 128], BF16, "band")
        S.dma("pool", ch_cp, [(wukv_sb[:], wukv), (poolw_sb[:], poolw), (band_sb[:], band)], [], [Bc])
        vec = C.sb([128, 16], F32, "vec")
        cst = C.sb([128, 3, 512], F32, "cst")
        S.dma("sp", ch_c, [(vec[:], vecs), (cst[:], consts)], [], [Bc])
        ident_bf = C.sb([128, 128], BF16, "identbf")
        tri_bf = C.sb([128, 128], BF16, "tribf")
        cmask = C.sb([128, 512], F32, "cmask")
        ones_bf = C.sb([128, 128], BF16, "onesbf")
        eps = C.sb([128, 1], F32, "eps")
        lbv = C.sb([128, 4], F32, "lbv")
        S.op("dve", [Bc], [Bc], lambda e: e.tensor_copy(out=ident_bf[:], in_=cst[:, 0, 0:128]))
        S.op("dve", [Bc], [Bc], lambda e: e.tensor_copy(out=tri_bf[:], in_=cst[:, 1, 0:128]))
        S.op("dve", [Bc], [Bc], lambda e: e.tensor_copy(out=cmask[:], in_=cst[:, 2, :]))
        S.op("dve", [], [Bc], lambda e: e.memset(ones_bf[:], 1.0))
        S.op("dve", [], [Bc], lambda e: e.memset(eps[:], EPS))
        mhalf = C.sb([128, 512], F32, "mhalf")
        S.op("dve", [], [Bc], lambda e: e.memset(mhalf[:], -0.5))
        S.op("dve", [Bc], [Bc], lambda e: e.tensor_tensor(out=lbv[:, 3:4], in0=vec[:, 2:3], in1=vec[:, 1:2],
                                                         op=ALU.subtract))
        S.op("act", [Bc], [Bc], lambda e: e.activation(out=lbv[:, 3:4], in_=lbv[:, 3:4], func=AF.Sigmoid))
        S.op("dve", [Bc], [Bc], lambda e: e.tensor_tensor(out=lbv[:, 0:1], in0=lbv[:, 3:4], in1=vec[:, 3:4],
                                                         op=ALU.mult))
        S.op("dve", [Bc], [Bc], lambda e: e.tensor_scalar(out=lbv[:, 1:2], in0=lbv[:, 0:1], scalar1=-1.0, scalar2=1.0,
                                                         op0=ALU.mult, op1=ALU.add))
        S.op("dve", [Bc], [Bc], lambda e: e.tensor_scalar(out=lbv[:, 2:3], in0=lbv[:, 1:2], scalar1=-1.0, scalar2=None,
                                                         op0=ALU.mult))
        KnT = C.sb([128, NS], BF16, "KnT")
        KpT = C.sb([128, NS], BF16, "KpT")
        S.op("pool", [], [Bc], lambda e: e.memset(KpT[64:128, :], 0.0))
        Vt = C.sb([128, NS // 128, 128], BF16, "Vt")
        B_K = [Buf("K%d" % t) for t in range(NBLK)]
        Sst = C.sb([128, 64], F32, "Sst")
        Sbf = C.sb([128, 2, 64], BF16, "Sbf")
        B_S = Buf("state")
        B_Sb = [Buf("sb0"), Buf("sb1")]
        S.op("dve", [], [B_S], lambda e: e.memset(Sst[:], 0.0))
        S.op("dve", [], B_Sb, lambda e: e.memset(Sbf[:], 0.0))
        tri8 = C.sb([64, 512], BF16, "tri8")
        for c_ in range(8):
            S.op("dve", [Bc], [Bc], lambda e: e.tensor_copy(out=tri8[:, c_ * 64:(c_ + 1) * 64], in_=cst[0:64, 1, 0:64]))
        khtr = C.sb_ring(2, [64, 1024], BF16, "kht")
        hring = C.sb_ring(2, [128, 8, 512], BF16, "hblk")
        ch_h = [S.chan("h0"), S.chan("h1")]
        csring = C.sb_ring(2, [64, 2, 512], F32, "cs")
        ch_cs = [S.chan("cs0"), S.chan("cs1")]
        pA = C.ps_ring(3, [128, 512], F32, "pA")
        pS = C.ps_ring(2, [128, 512], F32, "pS")
        pO = C.ps([128, 512], F32, "pO")
        pL = C.ps([128, 512], F32, "pL")
        B_pO, B_pL = Buf("pO"), Buf("pL")
        pH = C.ps([128, 512], F32, "pH")
        B_pH = Buf("pH")
        pX = C.ps([128, 512], F32, "pX") if False else None
        f32r = C.sb_ring(12, [128, 512], F32, "f")
        bfr = C.sb_ring(8, [128, 512], BF16, "b")
        ptr = C.sb_ring(3, [128, 512], BF16, "pt")
        qr = C.sb_ring(2, [128, 2, 512], BF16, "q")
        for (q_, Bq_) in qr.items:
            S.op("pool", [], [Bq_], lambda e: e.memset(q_[64:128, 1, :], 0.0))
        xpr = C.sb_ring(3, [128, 64], BF16, "xp")
        outr = C.sb_ring(2, [128, 3, 512], BF16, "out")
        ch_out = [S.chan("out0"), S.chan("out1")]
        prev_xp = None

        def rms_rstd(src_list, npart, nfeat):
            sq, B_sq = bfr.next()
            pss, B_pss = pA.next()
            for i, (ap, B) in enumerate(src_list):
                S.op("act", [B], [B_sq], lambda e: e.activation(out=sq[0:npart, :], in_=ap, func=AF.Square))
                S.op("pe", [B_sq, Bc], [B_pss],
                     lambda e: e.matmul(pss[0:npart, :], lhsT=ones_bf[0:npart, 0:npart], rhs=sq[0:npart, :],
                                        start=(i == 0), stop=(i == len(src_list) - 1)))
            rstd, B_r = f32r.next()
            S.op("act", [B_pss, Bc], [B_r],
                 lambda e: e.activation(out=rstd[0:npart, :], in_=pss[0:npart, :], func=AF.Ln, scale=1.0 / nfeat,
                                        bias=eps[0:npart, 0:1]))
            S.op("act", [B_r], [B_r],
                 lambda e: e.activation(out=rstd[0:npart, :], in_=rstd[0:npart, :], func=AF.Exp, scale=-0.5))
            return rstd, B_r

        def proj(hb, B_hb, c0, m):
            p, B_p = pA.next()
            for k in range(8):
                S.op("pe", [B_hb, Bc], [B_p],
                     lambda e: e.matmul(p[0:m, :], lhsT=win[:, k, c0:c0 + m], rhs=hb[:, k, :],
                                        start=(k == 0), stop=(k == 7)), inc=(k == 7))
            return p, B_p

        def prep_gen(t):
            nonlocal prev_xp
            bsl = slice(t * 512, (t + 1) * 512)
            hb, B_hb = hring.next()
            S.dma("sp", ch_h[t % 2], [(hb[:], hT[:, bsl].rearrange("(k p) t -> p k t", p=128))], [], [B_hb])
            cs, B_cs = csring.next()
            S.dma("sp", ch_cs[t % 2], [(cs[:], cs2[:, :, bsl])], [], [B_cs])
            ot, B_ot = outr.next()
            q, B_q = qr.next()

            pq0, B_pq0 = proj(hb, B_hb, 512, 128)
            yield
            pq1, B_pq1 = proj(hb, B_hb, 640, 128)
            rstd, B_r = rms_rstd([(pq0[:], B_pq0), (pq1[:], B_pq1)], 128, 256)
            yield
            cqn, B_cqn = bfr.next()
            cqn2, B_cqn2 = bfr.next()
            S.op("dve", [B_pq0, B_r, Bc], [B_cqn],
                 lambda e: e.scalar_tensor_tensor(out=cqn[:], in0=pq0[:], scalar=vec[:, 5:6], in1=rstd[:],
                                                  op0=ALU.mult, op1=ALU.mult))
            S.op("dve", [B_pq1, B_r, Bc], [B_cqn2],
                 lambda e: e.scalar_tensor_tensor(out=cqn2[:], in0=pq1[:], scalar=vec[:, 6:7], in1=rstd[:],
                                                  op0=ALU.mult, op1=ALU.mult))
            cq = [(cqn, B_cqn), (cqn2, B_cqn2)]
            yield
            pqn, B_pqn = pA.next()
            for c in range(2):
                S.op("pe", [cq[c][1], Bc], [B_pqn],
                     lambda e: e.matmul(pqn[:], lhsT=wuq_sb[:, c, 0:128], rhs=cq[c][0][:], start=(c == 0), stop=(c == 1)),
                     inc=(c == 1))
            S.op("act", [B_pqn], [B_q], lambda e: e.activation(out=q[:, 0, :], in_=pqn[:], func=AF.Copy, scale=SCALE))
            yield
            pqp, B_pqp = pA.next()
            for c in range(2):
                S.op("pe", [cq[c][1], Bc], [B_pqp],
                     lambda e: e.matmul(pqp[0:64, :], lhsT=wuq_sb[:, c, 128:192], rhs=cq[c][0][:], start=(c == 0),
                                        stop=(c == 1)), inc=(c == 1))
            pqr, B_pqr = pA.next()
            for c in range(2):
                S.op("pe", [cq[c][1], Bc], [B_pqr],
                     lambda e: e.matmul(pqr[0:64, :], lhsT=wuq_sb[:, c, 192:256], rhs=cq[c][0][:], start=(c == 0),
                                        stop=(c == 1)), inc=(c == 1))
            yield
            t1, B_t1 = f32r.next()
            t2, B_t2 = f32r.next()
            S.op("dve", [B_pqp, B_cs], [B_t1],
                 lambda e: e.scalar_tensor_tensor(out=t1[0:64, :], in0=pqp[0:64, :], scalar=SCALE, in1=cs[:, 0, :],
                                                  op0=ALU.mult, op1=ALU.mult))
            S.op("dve", [B_pqr, B_cs], [B_t2],
                 lambda e: e.scalar_tensor_tensor(out=t2[0:64, :], in0=pqr[0:64, :], scalar=SCALE, in1=cs[:, 1, :],
                                                  op0=ALU.mult, op1=ALU.mult))
            S.op("dve", [B_t1, B_t2], [B_q],
                 lambda e: e.tensor_tensor(out=q[0:64, 1, :], in0=t1[0:64, :], in1=t2[0:64, :], op=ALU.add))
            yield
            pkp, B_pkp = proj(hb, B_hb, 320, 64)
            pkr, B_pkr = proj(hb, B_hb, 384, 64)
            t1, B_t1 = f32r.next()
            t2, B_t2 = f32r.next()
            S.op("dve", [B_pkp, B_cs], [B_t1],
                 lambda e: e.tensor_tensor(out=t1[0:64, :], in0=pkp[0:64, :], in1=cs[:, 0, :], op=ALU.mult))
            S.op("dve", [B_pkr, B_cs], [B_t2],
                 lambda e: e.tensor_tensor(out=t2[0:64, :], in0=pkr[0:64, :], in1=cs[:, 1, :], op=ALU.mult))
            S.op("dve", [B_t1, B_t2], [B_K[t]],
                 lambda e: e.tensor_tensor(out=KpT[0:64, bsl], in0=t1[0:64, :], in1=t2[0:64, :], op=ALU.add))
            yield
            pkv, B_pkv = proj(hb, B_hb, 768, 128)
            rstd, B_r = rms_rstd([(pkv[:], B_pkv)], 128, 128)
            yield
            ckvn, B_ckvn = bfr.next()
            S.op("dve", [B_pkv, B_r, Bc], [B_ckvn],
                 lambda e: e.scalar_tensor_tensor(out=ckvn[:], in0=pkv[:], scalar=vec[:, 7:8], in1=rstd[:],
                                                  op0=ALU.mult, op1=ALU.mult))
            yield
            pkn, B_pkn = pA.next()
            S.op("pe", [B_ckvn, Bc], [B_pkn],
                 lambda e: e.matmul(pkn[:], lhsT=wukv_sb[:, 0:128], rhs=ckvn[:], start=True, stop=True))
            S.op("act", [B_pkn], [B_K[t]], lambda e: e.activation(out=KnT[:, bsl], in_=pkn[:], func=AF.Copy))
            yield
            pv, B_pv = pA.next()
            for s in range(4):
                S.op("pe", [B_ckvn, Bc], [B_pv],
                     lambda e: e.matmul(pv[:, s * 128:(s + 1) * 128], lhsT=ckvn[:, s * 128:(s + 1) * 128],
                                        rhs=wukv_sb[:, 128:256], start=True, stop=True), inc=(s == 3))
            S.op("act", [B_pv], [B_K[t]],
                 lambda e: e.activation(out=Vt[:, 4 * t:4 * t + 4, :],
                                        in_=pv[:].rearrange("p (s d) -> p s d", s=4), func=AF.Copy))

            yield
            phq, B_phq = proj(hb, B_hb, 0, 128)
            phf, B_phf = proj(hb, B_hb, 128, 128)
            yield
            phg, B_phg = proj(hb, B_hb, 256, 64)
            sig, B_sig = f32r.next()
            S.op("act", [B_phf], [B_sig], lambda e: e.activation(out=sig[:], in_=phf[:], func=AF.Sigmoid))
            qf, B_qf = f32r.next()
            S.op("act", [B_phq], [B_qf], lambda e: e.activation(out=qf[:], in_=phq[:], func=AF.Sigmoid))
            sg, B_sg = f32r.next()
            S.op("act", [B_phg], [B_sg], lambda e: e.activation(out=sg[0:64, :], in_=phg[0:64, :], func=AF.Sigmoid))
            pass
            yield
            S.op("dve", [B_phq, B_qf], [B_qf],
                 lambda e: e.tensor_tensor(out=qf[:], in0=phq[:], in1=qf[:], op=ALU.mult))
            S.op("dve", [B_phg, B_sg], [B_sg],
                 lambda e: e.tensor_tensor(out=sg[0:64, :], in0=phg[0:64, :], in1=sg[0:64, :], op=ALU.mult))
            lf, B_lf = f32r.next()
            S.op("dve", [B_sig, Bc], [B_lf],
                 lambda e: e.tensor_scalar(out=lf[:], in0=sig[:], scalar1=lbv[:, 1:2], scalar2=lbv[:, 0:1],
                                           op0=ALU.mult, op1=ALU.add))
            S.op("dve", [B_lf], [B_lf], lambda e: e.tensor_scalar_max(out=lf[:], in0=lf[:], scalar1=1e-20))
            S.op("act", [B_lf], [B_lf], lambda e: e.activation(out=lf[:], in_=lf[:], func=AF.Ln))
            yield
            kk, B_kk = f32r.next()
            S.op("dve", [B_sig, Bc], [B_kk],
                 lambda e: e.tensor_scalar(out=kk[:], in0=sig[:], scalar1=lbv[:, 2:3], scalar2=lbv[:, 1:2],
                                           op0=ALU.mult, op1=ALU.add))
            bb, B_bb = f32r.next()
            S.op("dve", [B_lf, Bc], [B_bb],
                 lambda e: e.tensor_tensor_scan(out=bb[:], data0=cmask[:], data1=lf[:], initial=0.0,
                                                op0=ALU.mult, op1=ALU.add))
            b3 = bb[:].rearrange("p (c j) -> p c j", j=64)
            yield
            d1, B_d1 = f32r.next()
            d2, B_d2 = f32r.next()
            S.op("dve", [B_bb], [B_d1],
                 lambda e: e.tensor_tensor(out=d1[:].rearrange("p (c j) -> p c j", j=64), in0=b3,
                                           in1=b3[:, :, 31:32].to_broadcast([128, 8, 64]), op=ALU.subtract))
            S.op("dve", [B_bb], [B_d2],
                 lambda e: e.tensor_tensor(out=d2[:].rearrange("p (c j) -> p c j", j=64), in0=b3,
                                           in1=b3[:, :, 63:64].to_broadcast([128, 8, 64]), op=ALU.subtract))
            yield
            e1, B_e1 = f32r.next()
            S.op("act", [B_d1], [B_e1], lambda e: e.activation(out=e1[:], in_=d1[:], func=AF.Exp))
            qt, B_qt = bfr.next()
            S.op("dve", [B_qf, B_e1], [B_qt], lambda e: e.tensor_tensor(out=qt[:], in0=qf[:], in1=e1[:], op=ALU.mult))
            S.op("act", [B_d1], [B_e1], lambda e: e.activation(out=e1[:], in_=d1[:], func=AF.Exp, scale=-1.0))
            yield
            kt, B_kt = bfr.next()
            S.op("dve", [B_kk, B_e1], [B_kt], lambda e: e.tensor_tensor(out=kt[:], in0=kk[:], in1=e1[:], op=ALU.mult))
            S.op("act", [B_d2], [B_d2], lambda e: e.activation(out=d2[:], in_=d2[:], func=AF.Exp, scale=-1.0))
            yield
            kh, B_kh = bfr.next()
            S.op("dve", [B_kk, B_d2], [B_kh], lambda e: e.tensor_tensor(out=kh[:], in0=kk[:], in1=d2[:], op=ALU.mult))
            S.op("act", [B_bb], [B_bb], lambda e: e.activation(out=bb[:], in_=bb[:], func=AF.Exp))
            yield
            qh, B_qh = bfr.next()
            S.op("dve", [B_qf, B_bb], [B_qh], lambda e: e.tensor_tensor(out=qh[:], in0=qf[:], in1=bb[:], op=ALU.mult))
            yield
            pV, B_pV = pA.next()
            for c in range(8):
                csl = slice(c * 64, (c + 1) * 64)
                for k in range(8):
                    S.op("pe", [B_hb, Bc], [B_pV],
                         lambda e: e.matmul(pV[0:64, csl], lhsT=hb[:, k, csl], rhs=win[:, k, 896:960],
                                            start=(k == 0), stop=(k == 7)), inc=(k == 7 and c == 7))
            vt, B_vt = bfr.next()
            S.op("act", [B_pV], [B_vt], lambda e: e.activation(out=vt[0:64, :], in_=pV[0:64, :], func=AF.Copy))
            yield
            pAT, B_pAT = pA.next()
            for c in range(8):
                csl = slice(c * 64, (c + 1) * 64)
                S.op("pe", [B_kt, B_qt], [B_pAT],
                     lambda e: e.matmul(pAT[0:64, csl], lhsT=kt[:, csl], rhs=qt[:, csl], start=True, stop=True),
                     inc=(c == 7))
            at, B_at = bfr.next()
            S.op("dve", [B_pAT, Bc], [B_at],
                 lambda e: e.tensor_tensor(out=at[0:64, :], in0=pAT[0:64, :], in1=tri8[0:64, :], op=ALU.mult))
            yield
            pKT, B_pKT = pA.next()
            pKTb = pKT[:].bitcast(BF16)
            for c in range(8):
                csl = slice(c * 64, (c + 1) * 64)
                S.op("pe", [B_kh, Bc], [B_pKT],
                     lambda e: e.transpose(out=pKTb[0:64, c * 128:(c + 1) * 128], in_=kh[:, csl], identity=ident_bf[:]),
                     inc=(c == 7))
            yield
            kht, B_kht = khtr.next()
            S.op("act", [B_pKT], [B_kht], lambda e: e.activation(out=kht[0:64, :], in_=pKTb[0:64, :], func=AF.Copy))
            for c in range(8):
                csl = slice(c * 64, (c + 1) * 64)
                S.op("pe", [B_kht, B_vt], [B_pH],
                     lambda e: e.matmul(pH[:, csl], lhsT=kht[0:64, c * 128:(c + 1) * 128], rhs=vt[0:64, csl],
                                        start=True, stop=True), inc=(c == 7))
            yield
            pOh, B_pOh = pA.next()
            for c in range(8):
                csl = slice(c * 64, (c + 1) * 64)
                S.op("pe", [B_vt, B_at], [B_pOh],
                     lambda e: e.matmul(pOh[0:64, csl], lhsT=vt[0:64, csl], rhs=at[0:64, csl], start=True, stop=False),
                     inc=False)
                S.op("pe", [B_Sb[c % 2], B_qh], [B_pOh],
                     lambda e: e.matmul(pOh[0:64, csl], lhsT=Sbf[:, c % 2, :], rhs=qh[:, csl], start=False, stop=True))
                S.op("dve", [B_pH, B_bb, B_S], [B_S],
                     lambda e: e.scalar_tensor_tensor(out=Sst[:], in0=Sst[:], scalar=bb[:, c * 64 + 63:c * 64 + 64],
                                                      in1=pH[:, csl], op0=ALU.mult, op1=ALU.add))
                S.op("dve", [B_S], [B_Sb[(c + 1) % 2]],
                     lambda e: e.tensor_copy(out=Sbf[:, (c + 1) % 2, :], in_=Sst[:]))
                yield
            oh, B_oh = f32r.next()
            S.op("act", [B_pOh], [B_oh], lambda e: e.activation(out=oh[0:64, :], in_=pOh[0:64, :], func=AF.Copy))
            yield
            rstd, B_r = rms_rstd([(oh[0:64, :], B_oh)], 64, 64)
            S.op("dve", [B_oh, B_r, Bc], [B_oh],
                 lambda e: e.scalar_tensor_tensor(out=oh[0:64, :], in0=oh[0:64, :], scalar=vec[0:64, 4:5],
                                                  in1=rstd[0:64, :], op0=ALU.mult, op1=ALU.mult))
            S.op("dve", [B_oh, B_sg], [B_ot],
                 lambda e: e.tensor_tensor(out=ot[0:64, 0, :], in0=oh[0:64, :], in1=sg[0:64, :], op=ALU.mult))

            yield
            ppool, B_pp = pA.next()
            for s in range(4):
                gt = 4 * t + s
                for k in range(8):
                    S.op("pe", [B_hb, Bc], [B_pH],
                         lambda e: e.matmul(pH[:, 384:448], lhsT=hb[:, k, s * 128:(s + 1) * 128], rhs=win[:, k, 960:1024],
                                            start=(k == 0), stop=(k == 7)), inc=(k == 7))
                xp, B_xp = xpr.next()
                S.op("act", [B_pH], [B_xp], lambda e: e.activation(out=xp[:], in_=pH[:, 384:448], func=AF.Copy))
                bi = 0 if gt == 0 else 1
                S.op("pe", [B_xp, Bc], [B_pp],
                     lambda e: e.matmul(ppool[0:64, s * 128:(s + 1) * 128], lhsT=xp[:], rhs=band_sb[:, bi, :],
                                        start=True, stop=(gt == 0)), inc=(gt == 0))
                if gt > 0:
                    pxp, B_pxp = prev_xp
                    S.op("pe", [B_pxp, Bc], [B_pp],
                         lambda e: e.matmul(ppool[0:64, s * 128:(s + 1) * 128], lhsT=pxp[:], rhs=band_sb[:, 2, :],
                                            start=False, stop=True))
                prev_xp = (xp, B_xp)
                yield
            yield
            pl_bf, B_plbf = bfr.next()
            S.op("act", [B_pp], [B_plbf], lambda e: e.activation(out=pl_bf[0:64, :], in_=ppool[0:64, :], func=AF.Copy))
            py, B_py = pA.next()
            S.op("pe", [B_plbf, Bc], [B_py],
                 lambda e: e.matmul(py[0:64, :], lhsT=poolw_sb[:, :], rhs=pl_bf[0:64, :], start=True, stop=True))
            S.op("act", [B_py, Bc], [B_ot],
                 lambda e: e.activation(out=ot[0:64, 2, :], in_=py[0:64, :], func=AF.Copy, scale=vec[0:64, 8:9]))

            yield (ot, B_ot, q, B_q)

        def attn(t, res, filler):
            ot, B_ot, q, B_q = res
            bsl = slice(t * 512, (t + 1) * 512)
            nkb = 4 * t + 4

            def qk(kb):
                j = kb - 4 * t
                q0 = 128 * j if j > 0 else 0
                nq = 512 - q0
                ksl = slice(kb * 128, (kb + 1) * 128)
                ps_, B_ps = pS.next()
                S.op("pe", [B_K[kb // 4], B_q], [B_ps],
                     lambda e: e.matmul(ps_[:, 0:nq], lhsT=KnT[:, ksl], rhs=q[:, 0, q0:512], start=True, stop=False),
                     inc=False)
                S.op("pe", [B_K[kb // 4], B_q], [B_ps],
                     lambda e: e.matmul(ps_[:, 0:nq], lhsT=KpT[:, ksl], rhs=q[:, 1, q0:512], start=False, stop=True))
                return ps_, B_ps, j, q0, nq

            nxt = qk(0)
            for kb in range(nkb):
                ps_, B_ps, j, q0, nq = nxt
                if kb + 1 < nkb:
                    nxt = qk(kb + 1)
                pt, B_pt = ptr.next()
                S.op("act", [B_ps], [B_pt], lambda e: e.activation(out=pt[:, 0:nq], in_=ps_[:, 0:nq], func=AF.Exp))
                if j >= 0:
                    S.op("dve", [B_pt, Bc], [B_pt],
                         lambda e: e.tensor_tensor(out=pt[:, 0:128], in0=pt[:, 0:128], in1=tri_bf[:], op=ALU.mult))
                S.op("pe", [B_K[kb // 4], B_pt], [B_pO],
                     lambda e: e.matmul(pO[:, q0:512], lhsT=Vt[:, kb, :], rhs=pt[:, 0:nq], start=(kb == 0),
                                        stop=(kb == nkb - 1)), inc=False)
                S.op("pe", [Bc, B_pt], [B_pO, B_pL],
                     lambda e: e.matmul(pL[:, q0:512], lhsT=ones_bf[:], rhs=pt[:, 0:nq], start=(kb == 0),
                                        stop=(kb == nkb - 1)))
                if filler is not None:
                    filler(kb, nkb)
            rl, B_rl = f32r.next()
            S.op("dve", [B_pL], [B_rl], lambda e: e.reciprocal(out=rl[:], in_=pL[:]))
            S.op("dve", [B_pO, B_rl], [B_ot],
                 lambda e: e.tensor_tensor(out=ot[:, 1, :], in0=pO[:], in1=rl[:], op=ALU.mult))
            S.dma("sp", ch_out[t % 2], [(oT[0:64, bsl], ot[0:64, 0, :]), (oT[64:192, bsl], ot[:, 1, :]),
                                        (oT[192:256, bsl], ot[0:64, 2, :])], [B_ot], [])

        def run_gen(g):
            r = None
            while True:
                try:
                    v = next(g)
                    if v is not None:
                        r = v
                except StopIteration:
                    return r

        NY = 56
        res = run_gen(prep_gen(0))
        for t in range(NBLK):
            nres = [None]
            if t + 1 < NBLK:
                g = prep_gen(t + 1)
                done = [False]

                def filler(kb, nkb, g=g, done=done, nres=nres):
                    if done[0]:
                        return
                    steps = ((kb + 1) * NY) // nkb - (kb * NY) // nkb
                    for _ in range(steps):
                        try:
                            v = next(g)
                            if v is not None:
                                nres[0] = v
                        except StopIteration:
                            done[0] = True
                            return
                attn(t, res, filler)
                if not done[0]:
                    r = run_gen(g)
                    if r is not None:
                        nres[0] = r
            else:
                attn(t, res, None)
            res = nres[0]
        S.finish("sp")
    return nc


POOL_WINDOWS = (2, 4, 8, 16)


def _gl(g):
    return np.ascontiguousarray(np.asarray(g, np.float32).reshape(8, 128).T)


def _rope_tables(ns):
    pos = np.arange(ns, dtype=np.float32)
    inv_freq = (1.0 / (np.float32(10000.0) ** (np.arange(0, 64, 2, dtype=np.float32) / np.float32(64)))).astype(np.float32)
    ang = (pos[:, None] * inv_freq[None, :]).astype(np.float32)
    cos = np.cos(ang).astype(np.float32).T
    sin = np.sin(ang).astype(np.float32).T
    cs2 = np.empty((64, 2, ns), np.float32)
    cs2[0:32, 0] = cos
    cs2[32:64, 0] = cos
    cs2[0:32, 1] = -sin
    cs2[32:64, 1] = sin
    return cs2


def _band(w):
    b = np.zeros((128, 3, 128), np.float32)
    s = np.arange(128)[:, None]
    t = np.arange(128)[None, :]
    win = (s <= t) & (s > t - w)
    eye = (s == t).astype(np.float32)
    cnt = np.minimum(t + 1, w).astype(np.float32)
    b[:, 0, :] = win / cnt - eye
    b[:, 1, :] = win / np.float32(w) - eye
    b[:, 2, :] = ((s - 128) > (t - w)).astype(np.float32) / np.float32(w)
    return b


def _consts():
    c = np.zeros((128, 3, 512), np.float32)
    c[:, 0, 0:128] = np.eye(128, dtype=np.float32)
    k = np.arange(128)[:, None]
    q = np.arange(128)[None, :]
    c[:, 1, 0:128] = (q >= k).astype(np.float32)
    c[:, 2, :] = 1.0
    c[:, 2, 0::64] = 0.0
    return c


def prep_B(inp, l, r, ns):
    w_in = np.asarray(inp["w_in"][l], np.float32)
    winh = np.zeros((D, 1024), np.float32)
    winh[:, 0:128] = w_in[:, r * 128:(r + 1) * 128]
    winh[:, 128:256] = w_in[:, 512 + r * 128:512 + (r + 1) * 128]
    winh[:, 256:320] = w_in[:, 1280 + r * 64:1280 + (r + 1) * 64]
    kpe = w_in[:, 1920:1984]
    winh[:, 320:384] = kpe
    winh[:, 384:416] = kpe[:, 32:64]
    winh[:, 416:448] = kpe[:, 0:32]
    winh[:, 512:768] = w_in[:, 1536:1792]
    winh[:, 768:896] = w_in[:, 1792:1920]
    winh[:, 896:960] = w_in[:, 1024 + r * 64:1024 + (r + 1) * 64]
    winh[:, 960:1024] = w_in[:, 1984 + r * 64:1984 + (r + 1) * 64]
    uq = np.asarray(inp["mla_w_uq"][l], np.float32)[:, r * 192:(r + 1) * 192]
    wuq = np.zeros((256, 256), np.float32)
    wuq[:, 0:192] = uq
    wuq[:, 192:224] = uq[:, 160:192]
    wuq[:, 224:256] = uq[:, 128:160]
    wukv = np.ascontiguousarray(np.asarray(inp["mla_w_ukv"][l], np.float32)[:, r * 256:(r + 1) * 256])
    poolw = np.ascontiguousarray(np.asarray(inp["pool_w"][l, r], np.float32))
    vecs = np.zeros((128, 16), np.float32)
    lbr = np.asarray(inp["hgrn_lower_bounds"], np.float32)
    vecs[:, 1] = lbr[0, r * 128:(r + 1) * 128]
    vecs[:, 2] = lbr[1, r * 128:(r + 1) * 128]
    vecs[:, 3] = 1.0 if l == 1 else 0.0
    vecs[0:64, 4] = np.asarray(inp["hgrn_out_norm"][l], np.float32)[r * 64:(r + 1) * 64]
    vecs[:, 5] = np.asarray(inp["mla_q_norm"][l], np.float32)[0:128]
    vecs[:, 6] = np.asarray(inp["mla_q_norm"][l], np.float32)[128:256]
    vecs[:, 7] = np.asarray(inp["mla_kv_norm"][l], np.float32)
    vecs[0:64, 8] = np.asarray(inp["pool_scale"][l], np.float32)[r * 64:(r + 1) * 64]
    return dict(winh=winh, wuq=wuq, wukv=wukv, poolw=poolw, vecs=vecs, cs2=_rope_tables(ns),
                band=_band(POOL_WINDOWS[r]), consts=_consts())


_PROGS = {}


def _prog(key, fn):
    if key not in _PROGS:
        _PROGS[key] = fn()
    return _PROGS[key]


def _wo_perm(w_o):
    idx = []
    for r in range(4):
        idx += list(range(r * 64, (r + 1) * 64))
        idx += list(range(256 + r * 128, 256 + (r + 1) * 128))
        idx += list(range(768 + r * 64, 768 + (r + 1) * 64))
    return np.ascontiguousarray(np.asarray(w_o, np.float32)[np.asarray(idx)])


def _run(nc, in_maps):
    res = run_bass_kernel_spmd(nc, in_maps, core_ids=list(range(NCORES)))
    return res.results


def kernel(**inp):
    inp = {k: np.asarray(v) for k, v in inp.items()}
    x = np.asarray(inp["x"], np.float32).reshape(BATCH * SEQ, D)
    ident = np.eye(128, dtype=np.float32)
    ncA = _prog("A", build_A)
    gv = np.concatenate([_gl(inp["attn_norm"][0]), _gl(inp["attn_norm"][0])], axis=1)
    xTs = [np.ascontiguousarray(x[c * TOK:(c + 1) * TOK].T) for c in range(NCORES)]
    rA = _run(ncA, [dict(xT=xTs[c], gvec=gv) for c in range(NCORES)])
    hTs = [rA[c]["hT"] for c in range(NCORES)]
    out = None
    for l in range(2):
        ncB = _prog("B", build_B)
        hfull = [np.ascontiguousarray(np.concatenate(hTs[4 * b:4 * b + 4], axis=1)) for b in range(BATCH)]
        insB = []
        for c in range(NCORES):
            d = prep_B(inp, l, c % 4, SEQ)
            d["hT"] = hfull[c // 4]
            insB.append(d)
        rB = _run(ncB, insB)
        wo = _wo_perm(inp["w_o"][l])
        insC = []
        for c in range(NCORES):
            b, qd = c // 4, c % 4
            oT = np.ascontiguousarray(np.concatenate(
                [rB[4 * b + r]["oTo"][:, qd * TOK:(qd + 1) * TOK] for r in range(4)], axis=0))
            d = dict(xT=xTs[c], oT=oT, wo=wo, ident=ident)
            if l == 0:
                d["gvec"] = np.concatenate([_gl(inp["ffn_norm"][0]), _gl(inp["attn_norm"][1])], axis=1)
                d["wg"] = np.asarray(inp["dense_w_gate"][0], np.float32)
                d["wu"] = np.asarray(inp["dense_w_up"][0], np.float32)
                d["wd"] = np.asarray(inp["dense_w_down"][0], np.float32)
            else:
                d["gvec"] = np.concatenate([_gl(inp["ffn_norm"][1]), _gl(inp["final_norm"])], axis=1)
                d["wg"] = np.asarray(inp["moe_w_gate"][0], np.float32)
                d["wu"] = np.asarray(inp["moe_w_up"][0], np.float32)
                d["wd"] = np.asarray(inp["moe_w_down"][0], np.float32)
                d["router"] = np.asarray(inp["moe_router"][0], np.float32)
            insC.append(d)
        if l == 0:
            rC = _run(_prog("C0", lambda: build_C("dense", False)), insC)
            xTs = [rC[c]["x2T"] for c in range(NCORES)]
            hTs = [rC[c]["hnT"] for c in range(NCORES)]
        else:
            rC = _run(_prog("C1", lambda: build_C("moe", True)), insC)
            out = np.concatenate([rC[c]["outT"].T for c in range(NCORES)], axis=0)
    return np.ascontiguousarray(out.reshape(BATCH, SEQ, D).astype(np.float32))
```
